# Optimizing a Trainium2 kernel written in Bass

```python
import jax, jax.numpy as jnp
from jax import lax
import numpy as np

D_MODEL = 2048
BATCH = 2
SEQ = 4096
DEPTH = 1

GRID_W = 64
CTX_LEN = 256
NORM_EPS = 1e-6

NA_HEADS = 8
NA_HEAD_DIM = 128
NA_WIN_H = 8
NA_WIN_W = 16

GLA_HEADS = 4
GLA_DK = 128
GLA_DV = 256
GLA_GATE_RANK = 16
GLA_GATE_TAU = 16.0
GLA_CHUNK = 64
ROPE_BASE = 10000.0

N_EXPERTS = 64
N_GROUPS = 8
TOPK_GROUPS = 4
TOP_K = 6
D_EXPERT = 512
ROUTED_SCALE = 2.5
MOE_BLOCK = 128

NA_WIDTH = NA_HEADS * NA_HEAD_DIM
GLA_KEY_WIDTH = GLA_HEADS * GLA_DK
GLA_VAL_WIDTH = GLA_HEADS * GLA_DV
MIX_WIDTH = NA_WIDTH + GLA_VAL_WIDTH
SPLIT_SIZES = (NA_WIDTH, NA_WIDTH, NA_WIDTH, GLA_KEY_WIDTH, GLA_KEY_WIDTH,
               GLA_VAL_WIDTH, GLA_VAL_WIDTH, GLA_GATE_RANK, GLA_GATE_RANK)
IN_COLS = sum(SPLIT_SIZES)

kernel_name = 'hybrid_na_gla_moe_dit_layer'


def rms_norm(x, g):
    xf = x.astype(jnp.float32)
    y = xf * lax.rsqrt(jnp.mean(xf * xf, axis=-1, keepdims=True) + NORM_EPS)
    return (y * g.astype(jnp.float32)).astype(x.dtype)


def adaln_params(cond, w_mod, b_mod):
    return jax.nn.silu(cond) @ w_mod + b_mod


def modulate(h, shift, scale):
    return h * (1.0 + scale) + shift


def split_columns(p):
    points, acc = [], 0
    for s in SPLIT_SIZES[:-1]:
        acc += s
        points.append(acc)
    return jnp.split(p, points, axis=-1)


def to_heads(t, n_heads, d_head):
    return t.reshape(t.shape[0], t.shape[1], n_heads, d_head).transpose(0, 2, 1, 3)


def from_heads(t):
    b, n, l, d = t.shape
    return t.transpose(0, 2, 1, 3).reshape(b, l, n * d)


def rope_1d(x, pos):
    half = x.shape[-1] // 2
    inv_freq = ROPE_BASE ** (-jnp.arange(half, dtype=jnp.float32) / half)
    ang = pos.astype(jnp.float32)[:, None] * inv_freq[None, :]
    cos = jnp.cos(ang)[None, :, None, :]
    sin = jnp.sin(ang)[None, :, None, :]
    x1, x2 = x[..., :half], x[..., half:]
    return jnp.concatenate([x1 * cos - x2 * sin, x2 * cos + x1 * sin], axis=-1)


def axial_rope(x, row_pos, col_pos):
    xf = x.astype(jnp.float32)
    half = x.shape[-1] // 2
    return jnp.concatenate([rope_1d(xf[..., :half], row_pos),
                            rope_1d(xf[..., half:], col_pos)], axis=-1).astype(x.dtype)


def neighbourhood_attention(q, k, v, k_ctx, v_ctx, rpb):
    b, h, l, dh = q.shape
    rows = l // GRID_W
    kh = min(NA_WIN_H, rows)
    kw = NA_WIN_W
    scale = dh ** -0.5
    qg = q.reshape(b, h, rows, GRID_W, dh).transpose(2, 0, 1, 3, 4)
    kg = k.reshape(b, h, rows, GRID_W, dh)
    vg = v.reshape(b, h, rows, GRID_W, dh)
    col = jnp.arange(GRID_W)
    col_start = jnp.clip(col - kw // 2, 0, GRID_W - kw)
    col_idx = col_start[:, None] + jnp.arange(kw)[None, :]
    dc = col_idx - col[:, None] + (kw - 1)
    rpb_cols = rpb[:, :, dc]

    def row_block(args):
        r, q_r = args
        rs = jnp.clip(r - kh // 2, 0, rows - kh)
        k_rows = lax.dynamic_slice_in_dim(kg, rs, kh, axis=2)
        v_rows = lax.dynamic_slice_in_dim(vg, rs, kh, axis=2)
        k_win = k_rows[:, :, :, col_idx, :]
        v_win = v_rows[:, :, :, col_idx, :]
        dr = rs + jnp.arange(kh) - r + (NA_WIN_H - 1)
        bias = jnp.take(rpb_cols, dr, axis=1).transpose(0, 2, 1, 3)
        s_loc = jnp.einsum('bhqd,bhiqjd->bhqij', q_r, k_win).astype(jnp.float32) * scale
        s_loc = s_loc + bias.astype(jnp.float32)[None]
        s_ctx = jnp.einsum('bhqd,bhcd->bhqc', q_r, k_ctx).astype(jnp.float32) * scale
        logits = jnp.concatenate([s_loc.reshape(b, h, GRID_W, kh * kw), s_ctx], axis=-1)
        p = jax.nn.softmax(logits, axis=-1).astype(v.dtype)
        p_loc = p[..., :kh * kw].reshape(b, h, GRID_W, kh, kw)
        p_ctx = p[..., kh * kw:]
        return (jnp.einsum('bhqij,bhiqjd->bhqd', p_loc, v_win)
                + jnp.einsum('bhqc,bhcd->bhqd', p_ctx, v_ctx))

    o = lax.map(row_block, (jnp.arange(rows), qg))
    return o.transpose(1, 2, 0, 3, 4).reshape(b, h, l, dh)


def context_attention(q, k, v):
    s = jnp.einsum('bhqd,bhcd->bhqc', q, k).astype(jnp.float32) * (q.shape[-1] ** -0.5)
    p = jax.nn.softmax(s, axis=-1).astype(v.dtype)
    return jnp.einsum('bhqc,bhcd->bhqd', p, v)


def gla_chunk_scan(k, v, g, s0, q=None):
    b, h, l, dk = k.shape
    dv = v.shape[-1]
    n = l // GLA_CHUNK
    mask = jnp.tril(jnp.ones((GLA_CHUNK, GLA_CHUNK), dtype=bool))

    def to_chunks(t):
        return t.reshape(b, h, n, GLA_CHUNK, t.shape[-1]).transpose(2, 0, 1, 3, 4)

    def step(state, xs):
        if q is None:
            kc, vc, gc = xs
            qc = None
        else:
            qc, kc, vc, gc = xs
        cum = jnp.cumsum(gc, axis=2)
        cum_last = cum[:, :, -1:, :]
        new_state = (jnp.exp(cum_last[:, :, 0, :, None]) * state
                     + jnp.einsum('bhsd,bhsv->bhdv', kc * jnp.exp(cum_last - cum), vc))
        if qc is None:
            return new_state, None
        diff = cum[:, :, :, None, :] - cum[:, :, None, :, :]
        decay = jnp.exp(jnp.where(mask[:, :, None], diff, -jnp.inf))
        att = jnp.einsum('bhtd,bhsd,bhtsd->bhts', qc, kc, decay)
        out = (jnp.einsum('bhts,bhsv->bhtv', att, vc)
               + jnp.einsum('bhtd,bhdv->bhtv', qc * jnp.exp(cum), state))
        return new_state, out

    seqs = (k, v, g) if q is None else (q, k, v, g)
    state, out = lax.scan(step, s0, tuple(to_chunks(t) for t in seqs))
    if q is None:
        return None, state
    return out.transpose(1, 2, 0, 3, 4).reshape(b, h, l, dv), state


def gla_bidirectional(q_l, k_l, v_l, gf_l, gb_l, q_c, k_c, v_c, gf_c, gb_c):
    b, h, _, dk = k_l.shape
    dv = v_l.shape[-1]
    s0 = jnp.zeros((b, h, dk, dv), jnp.float32)

    def rev(t):
        return None if t is None else jnp.flip(t, axis=2)

    o_cf, s_cf = gla_chunk_scan(k_c, v_c, gf_c, s0, q_c)
    o_lf, _ = gla_chunk_scan(k_l, v_l, gf_l, s_cf, q_l)
    o_cb, s_cb = gla_chunk_scan(rev(k_c), rev(v_c), rev(gb_c), s0, rev(q_c))
    o_lb, _ = gla_chunk_scan(rev(k_l), rev(v_l), rev(gb_l), s_cb, rev(q_l))
    o_l = o_lf + rev(o_lb)
    o_c = None if q_c is None else o_cf + rev(o_cb)
    return o_l, o_c


def token_mixers(h_lat, h_ctx, w_in, q_norm_g, k_norm_g, na_rpb, gla_gate_up_f, gla_gate_bias_f,
                 gla_gate_up_b, gla_gate_bias_b, gla_norm_g, w_out, need_ctx):
    b, l, _ = h_lat.shape
    p_l = split_columns(h_lat @ w_in)
    p_c = split_columns(h_ctx @ w_in)

    def na_qkv(p):
        bb, ll = p[0].shape[:2]
        q = rms_norm(p[0].reshape(bb, ll, NA_HEADS, NA_HEAD_DIM), q_norm_g)
        k = rms_norm(p[1].reshape(bb, ll, NA_HEADS, NA_HEAD_DIM), k_norm_g)
        v = p[2].reshape(bb, ll, NA_HEADS, NA_HEAD_DIM)
        return q.transpose(0, 2, 1, 3), k.transpose(0, 2, 1, 3), v.transpose(0, 2, 1, 3)

    nq_l, nk_l, nv_l = na_qkv(p_l)
    nq_c, nk_c, nv_c = na_qkv(p_c)
    o_na_l = from_heads(neighbourhood_attention(nq_l, nk_l, nv_l, nk_c, nv_c, na_rpb))

    t = jnp.arange(l)
    row_pos = t // GRID_W
    col_pos = t % GRID_W
    q_scale = GLA_DK ** -0.5

    def gla_qk(p, positioned):
        bb, ll = p[3].shape[:2]
        q = p[3].reshape(bb, ll, GLA_HEADS, GLA_DK)
        k = p[4].reshape(bb, ll, GLA_HEADS, GLA_DK)
        if positioned:
            q = axial_rope(q, row_pos, col_pos)
            k = axial_rope(k, row_pos, col_pos)
        q = (q.astype(jnp.float32) * q_scale).transpose(0, 2, 1, 3)
        k = k.astype(jnp.float32).transpose(0, 2, 1, 3)
        return q, k

    def log_decay(down, up, bias):
        z = (down @ up + bias).astype(jnp.float32)
        return to_heads(jax.nn.log_sigmoid(z) / GLA_GATE_TAU, GLA_HEADS, GLA_DK)

    def gla_values(p):
        return to_heads(p[5].astype(jnp.float32), GLA_HEADS, GLA_DV)

    def gla_out(o, r):
        o = rms_norm(o.transpose(0, 2, 1, 3), gla_norm_g)
        return o.reshape(o.shape[0], o.shape[1], GLA_VAL_WIDTH).astype(r.dtype) * jax.nn.silu(r)

    gq_l, gk_l = gla_qk(p_l, True)
    gq_c, gk_c = gla_qk(p_c, False)
    o_gla_l, o_gla_c = gla_bidirectional(
        gq_l, gk_l, gla_values(p_l),
        log_decay(p_l[7], gla_gate_up_f, gla_gate_bias_f), log_decay(p_l[8], gla_gate_up_b, gla_gate_bias_b),
        gq_c if need_ctx else None, gk_c, gla_values(p_c),
        log_decay(p_c[7], gla_gate_up_f, gla_gate_bias_f), log_decay(p_c[8], gla_gate_up_b, gla_gate_bias_b))

    out_l = jnp.concatenate([o_na_l, gla_out(o_gla_l, p_l[6])], axis=-1) @ w_out
    if not need_ctx:
        return out_l, None
    o_na_c = from_heads(context_attention(nq_c, nk_c, nv_c))
    out_c = jnp.concatenate([o_na_c, gla_out(o_gla_c, p_c[6])], axis=-1) @ w_out
    return out_l, out_c


def moe_ffn(h, router_w, router_bias, w1, w3, w2, ws1, ws3, ws2):
    shape = h.shape
    tok = h.reshape(-1, shape[-1])
    n_tok = tok.shape[0]
    scores = jax.nn.sigmoid((tok @ router_w).astype(jnp.float32))
    biased = scores + router_bias.astype(jnp.float32)
    per_group = N_EXPERTS // N_GROUPS
    group_score = lax.top_k(biased.reshape(n_tok, N_GROUPS, per_group), 2)[0].sum(axis=-1)
    _, gidx = lax.top_k(group_score, TOPK_GROUPS)
    gmask = jnp.sum(jax.nn.one_hot(gidx, N_GROUPS, dtype=jnp.float32), axis=1) > 0
    emask = jnp.repeat(gmask, per_group, axis=1)
    _, eidx = lax.top_k(jnp.where(emask, biased, -jnp.inf), TOP_K)
    sel = jnp.take_along_axis(scores, eidx, axis=1)
    wts = sel / jnp.sum(sel, axis=-1, keepdims=True) * ROUTED_SCALE
    combine = jnp.einsum('tke,tk->te', jax.nn.one_hot(eidx, N_EXPERTS, dtype=jnp.float32), wts).astype(h.dtype)

    def expert_block(args):
        tb, cb = args
        act = jax.nn.silu(jnp.einsum('td,edf->tef', tb, w1)) * jnp.einsum('td,edf->tef', tb, w3)
        return jnp.einsum('tef,efd->td', act * cb[:, :, None], w2)

    n_blk = n_tok // MOE_BLOCK
    routed = lax.map(expert_block, (tok.reshape(n_blk, MOE_BLOCK, shape[-1]),
                                    combine.reshape(n_blk, MOE_BLOCK, N_EXPERTS))).reshape(n_tok, shape[-1])
    shared = (jax.nn.silu(tok @ ws1) * (tok @ ws3)) @ ws2
    return (routed + shared).reshape(shape)


def setup_inputs(seed: int = 0) -> dict:
    key = jax.random.key(seed)
    ks = jax.random.split(key, 26)

    def nrm(k, shape, scale):
        return jax.random.normal(k, shape, jnp.float32) * scale

    return {
        'x': nrm(ks[0], (BATCH, SEQ, D_MODEL), 1.0),
        'c': nrm(ks[1], (BATCH, D_MODEL), 1.0),
        'ctx': nrm(ks[2], (BATCH, CTX_LEN, D_MODEL), 1.0),
        'c_ctx': nrm(ks[3], (D_MODEL,), 1.0),
        'w_mod': nrm(ks[4], (DEPTH, D_MODEL, 6 * D_MODEL), 0.5 * D_MODEL ** -0.5),
        'b_mod': nrm(ks[5], (DEPTH, 6 * D_MODEL), 0.02),
        'norm1_g': 1.0 + nrm(ks[6], (DEPTH, D_MODEL), 0.02),
        'norm2_g': 1.0 + nrm(ks[7], (DEPTH, D_MODEL), 0.02),
        'w_in': nrm(ks[8], (DEPTH, D_MODEL, IN_COLS), D_MODEL ** -0.5),
        'q_norm_g': 1.0 + nrm(ks[9], (DEPTH, NA_HEAD_DIM), 0.02),
        'k_norm_g': 1.0 + nrm(ks[10], (DEPTH, NA_HEAD_DIM), 0.02),
        'na_rpb': nrm(ks[11], (DEPTH, NA_HEADS, 2 * NA_WIN_H - 1, 2 * NA_WIN_W - 1), 0.1),
        'gla_gate_up_f': nrm(ks[12], (DEPTH, GLA_GATE_RANK, GLA_KEY_WIDTH), GLA_GATE_RANK ** -0.5),
        'gla_gate_bias_f': nrm(ks[13], (DEPTH, GLA_KEY_WIDTH), 0.1),
        'gla_gate_up_b': nrm(ks[14], (DEPTH, GLA_GATE_RANK, GLA_KEY_WIDTH), GLA_GATE_RANK ** -0.5),
        'gla_gate_bias_b': nrm(ks[15], (DEPTH, GLA_KEY_WIDTH), 0.1),
        'gla_norm_g': 1.0 + nrm(ks[16], (DEPTH, GLA_DV), 0.02),
        'w_out': nrm(ks[17], (DEPTH, MIX_WIDTH, D_MODEL), MIX_WIDTH ** -0.5),
        'router_w': nrm(ks[18], (DEPTH, D_MODEL, N_EXPERTS), D_MODEL ** -0.5),
        'router_bias': nrm(ks[19], (DEPTH, N_EXPERTS), 0.01),
        'expert_w1': nrm(ks[20], (DEPTH, N_EXPERTS, D_MODEL, D_EXPERT), D_MODEL ** -0.5),
        'expert_w3': nrm(ks[21], (DEPTH, N_EXPERTS, D_MODEL, D_EXPERT), D_MODEL ** -0.5),
        'expert_w2': nrm(ks[22], (DEPTH, N_EXPERTS, D_EXPERT, D_MODEL), D_EXPERT ** -0.5),
        'shared_w1': nrm(ks[23], (DEPTH, D_MODEL, D_EXPERT), D_MODEL ** -0.5),
        'shared_w3': nrm(ks[24], (DEPTH, D_MODEL, D_EXPERT), D_MODEL ** -0.5),
        'shared_w2': nrm(ks[25], (DEPTH, D_EXPERT, D_MODEL), D_EXPERT ** -0.5),
    }


def reference(x, c, ctx, c_ctx, w_mod, b_mod, norm1_g, norm2_g, w_in, q_norm_g, k_norm_g, na_rpb,
              gla_gate_up_f, gla_gate_bias_f, gla_gate_up_b, gla_gate_bias_b, gla_norm_g, w_out,
              router_w, router_bias, expert_w1, expert_w3, expert_w2, shared_w1, shared_w3, shared_w2):
    for layer in range(DEPTH):
        need_ctx = layer < DEPTH - 1
        sh1, sc1, g1, sh2, sc2, g2 = jnp.split(
            adaln_params(c, w_mod[layer], b_mod[layer])[:, None, :], 6, axis=-1)
        csh1, csc1, cg1, csh2, csc2, cg2 = jnp.split(
            adaln_params(c_ctx, w_mod[layer], b_mod[layer]), 6, axis=-1)
        h_l = modulate(rms_norm(x, norm1_g[layer]), sh1, sc1)
        h_c = modulate(rms_norm(ctx, norm1_g[layer]), csh1, csc1)
        o_l, o_c = token_mixers(h_l, h_c, w_in[layer], q_norm_g[layer], k_norm_g[layer], na_rpb[layer],
                                gla_gate_up_f[layer], gla_gate_bias_f[layer], gla_gate_up_b[layer],
                                gla_gate_bias_b[layer], gla_norm_g[layer], w_out[layer], need_ctx)
        x = x + g1 * o_l
        x = x + g2 * moe_ffn(modulate(rms_norm(x, norm2_g[layer]), sh2, sc2), router_w[layer],
                             router_bias[layer], expert_w1[layer], expert_w3[layer], expert_w2[layer],
                             shared_w1[layer], shared_w3[layer], shared_w2[layer])
        if need_ctx:
            ctx = ctx + cg1 * o_c
            ctx = ctx + cg2 * moe_ffn(modulate(rms_norm(ctx, norm2_g[layer]), csh2, csc2), router_w[layer],
                                      router_bias[layer], expert_w1[layer], expert_w3[layer],
                                      expert_w2[layer], shared_w1[layer], shared_w3[layer],
                                      shared_w2[layer])
    return x
```

```python
import numpy as np
from contextlib import ExitStack
import concourse.bass as bass
import concourse.mybir as mybir
from concourse.bass_utils import run_bass_kernel_spmd

F32 = mybir.dt.float32
BF16 = mybir.dt.bfloat16
AF = mybir.ActivationFunctionType
ALU = mybir.AluOpType
AX = mybir.AxisListType

D = 2048
KC = 16
NEG = -30000.0
EPS = 1e-6
DEBUG = False
N_EXP = 64
PHASE_MARKS = []


class Sched:
    CE = ("pe", "act", "dve", "pool")

    def __init__(self, nc, stack):
        self.nc = nc
        self.stack = stack
        self.ops = {k: [] for k in ("pe", "act", "dve", "pool", "sp")}
        self.cnt = {}
        self.seen = {k: {} for k in self.ops}
        self.sems = {}
        self.gen = 0
        self.cur = {}
        self.pending = {k: False for k in self.ops}
        self._new_compute_sems()
        self.qsems = {"sp": [], "pool": [], "act": []}
        self.qnext = {"sp": 0, "pool": 0, "act": 0}
        for q, n in (("sp", 16), ("pool", 12), ("act", 4)):
            for i in range(n):
                k = "d_%s%d" % (q, i)
                self.sems[k] = stack.enter_context(nc.semaphore(k))
                self.cnt[k] = 0
                self.qsems[q].append(k)

    def _new_compute_sems(self):
        for e in self.CE:
            k = "c_%s%d" % (e, self.gen)
            self.sems[k] = self.stack.enter_context(self.nc.semaphore(k))
            self.cnt[k] = 0
            self.cur[e] = k
        self.gen += 1

    @staticmethod
    def res():
        return {"w": None, "r": {}}

    def _deps(self, eng, reads, writes):
        deps = {}

        def add(k, c):
            if deps.get(k, 0) < c:
                deps[k] = c
        for r in reads:
            if r["w"] is not None:
                add(*r["w"])
        for w in writes:
            if w["w"] is not None:
                add(*w["w"])
            for k, c in w["r"].items():
                add(k, c)
        out = []
        for k, c in deps.items():
            if k == self.cur.get(eng) and c > self.cnt[k]:
                continue
            if self.seen[eng].get(k, 0) < c:
                self.seen[eng][k] = c
                out.append((k, c))
        return out

    def _mark(self, tok, reads, writes):
        k, c = tok
        for r in reads:
            if r["r"].get(k, 0) < c:
                r["r"][k] = c
        for w in writes:
            w["w"] = tok
            w["r"] = {}

    def op(self, eng, fn, reads=(), writes=(), signal=True):
        waits = self._deps(eng, reads, writes)
        k = self.cur[eng]
        if signal:
            self.cnt[k] += 1
            tok = (k, self.cnt[k])
            self.ops[eng].append((waits, fn, (k, 1)))
            self.pending[eng] = False
        else:
            tok = (k, self.cnt[k] + 1)
            self.ops[eng].append((waits, fn, None))
            self.pending[eng] = True
        self._mark(tok, reads, writes)
        return tok

    def dma(self, q, fn, reads=(), writes=()):
        sl = self.qsems[q]
        k = sl[self.qnext[q] % len(sl)]
        self.qnext[q] += 1
        waits = self._deps(q, reads, writes)
        prev = self.cnt[k]
        if prev > 0 and self.seen[q].get(k, 0) < prev:
            self.seen[q][k] = prev
            waits.append((k, prev))
        self.cnt[k] += 16
        tok = (k, self.cnt[k])
        self.ops[q].append((waits, fn, (k, 16)))
        self._mark(tok, reads, writes)
        return tok

    def barrier(self):
        assert not any(self.pending.values()), self.pending
        targets = [(k, c) for k, c in self.cnt.items() if c > 0]
        for e in self.ops:
            waits = []
            for k, c in targets:
                if self.seen[e].get(k, 0) < c:
                    self.seen[e][k] = c
                    waits.append((k, c))
            if waits:
                self.ops[e].append((waits, None, None))
        self._new_compute_sems()

    def emit(self, block):
        sems = self.sems

        def runner(name):
            def f(e):
                for waits, fn, inc in self.ops[name]:
                    for k, c in waits:
                        e.wait_ge(sems[k], c)
                    if fn is not None:
                        ins = fn(e)
                        if inc is not None:
                            ins.then_inc(sems[inc[0]], inc[1])
            return f
        block.tensor(runner("pe"))
        block.scalar(runner("act"))
        block.vector(runner("dve"))
        block.gpsimd(runner("pool"))
        block.sync(runner("sp"))


class Buf:
    def __init__(self, ap):
        self.ap = ap
        self.r = Sched.res()


def _swap_perm():
    idx = np.arange(128).reshape(2, 2, 32)
    return idx[:, ::-1, :].reshape(128)


def _rope_tables(pos_row, pos_col):
    inv = (10000.0 ** (-np.arange(32, dtype=np.float32) / 32.0)).astype(np.float32)
    ar = pos_row.astype(np.float32)[:, None] * inv[None, :]
    ac = pos_col.astype(np.float32)[:, None] * inv[None, :]
    cr, sr = np.cos(ar), np.sin(ar)
    cc, sc = np.cos(ac), np.sin(ac)
    cos = np.concatenate([cr, cr, cc, cc], axis=1).astype(np.float32)
    sin = np.concatenate([-sr, sr, -sc, sc], axis=1).astype(np.float32)
    return cos, sin


def _halo_chunks(q):
    hb = [8 * q - 2, 8 * q - 1] if q > 0 else [None, 3]
    ha = [8 * q + 8, 8 * q + 9] if q < 3 else [28, None]
    return hb + ha


def _na_bias(q, rpb):
    halo = _halo_chunks(q)
    out = np.full((8, 8, 128, 5, 128), NEG, np.float32)
    pk = np.arange(128)
    for i in range(8):
        tq = (8 * q + i) * 128 + np.arange(128)
        r, cq = tq // 64, tq % 64
        rs = np.clip(r - 4, 0, 56)
        cs = np.clip(cq - 8, 0, 48)
        seen = set()
        order = sorted(range(5), key=lambda j: (not (0 <= i - 2 + j <= 7), j))
        for j in order:
            sl = i - 2 + j
            if 0 <= sl <= 7:
                g = 8 * q + sl
            else:
                g = halo[{-2: 0, -1: 1, 8: 2, 9: 3}[sl]]
            if g is None or g in seen:
                continue
            seen.add(g)
            tk = g * 128 + pk
            rk, ck = tk // 64, tk % 64
            valid = ((rk[:, None] >= rs[None, :]) & (rk[:, None] < rs[None, :] + 8) &
                     (ck[:, None] >= cs[None, :]) & (ck[:, None] < cs[None, :] + 16))
            dr = np.clip(rk[:, None] - r[None, :] + 7, 0, 14)
            dc = np.clip(ck[:, None] - cq[None, :] + 15, 0, 30)
            vals = rpb[:, dr, dc]
            out[:, i, :, j, :] = np.where(valid[None], vals, NEG)
    return out.reshape(8, 8, 128, 640)


def _wlay(w, cw):
    n = w.shape[1]
    return np.ascontiguousarray(w.reshape(16, 128, n // cw, cw).transpose(2, 1, 0, 3))


def _prep_shared(inp):
    sh = {}
    w_in = inp["w_in"][0]
    o = np.cumsum([0, 1024, 1024, 1024, 512, 512, 1024, 1024, 16, 16])
    naq, nak, nav, gq, gk, gv, gg, df, db = [w_in[:, o[i]:o[i + 1]] for i in range(9)]
    perm = np.concatenate([h * 128 + _swap_perm() for h in range(4)])
    wdn = np.zeros((2048, 128), np.float32)
    wdn[:, 0:16] = df
    wdn[:, 32:48] = db
    wA = np.concatenate([naq, nak, gq, gq[:, perm], gk, gk[:, perm], gg, wdn, nav, gv], axis=1)
    sh["wA"] = _wlay(wA, 128)
    wB = np.concatenate([gk, gv], axis=1)
    sh["wB"] = _wlay(wB, 512)
    sh["wdn"] = np.ascontiguousarray(wdn[:, 0:64].reshape(16, 128, 64).transpose(1, 0, 2))
    sh["wmod"] = _wlay(inp["w_mod"][0], 512)
    sh["bmod2"] = np.ascontiguousarray(np.broadcast_to(inp["b_mod"][0][None], (2, 12288)))
    sh["n1g2"] = np.ascontiguousarray(np.broadcast_to(inp["norm1_g"][0][None], (2, 2048)))
    sh["n2g2"] = np.ascontiguousarray(np.broadcast_to(inp["norm2_g"][0][None], (2, 2048)))
    UP = np.zeros((64, 512), np.float32)
    UP[0:16] = inp["gla_gate_up_f"][0]
    UP[16] = inp["gla_gate_bias_f"][0]
    UP[32:48] = inp["gla_gate_up_b"][0]
    UP[48] = inp["gla_gate_bias_b"][0]
    sh["UP"] = UP
    sv = np.zeros((128, 8), np.float32)
    sv[:, 0] = inp["q_norm_g"][0]
    sv[:, 1] = inp["k_norm_g"][0]
    sv[:, 2] = inp["gla_norm_g"][0][0:128]
    sv[:, 3] = inp["gla_norm_g"][0][128:256]
    sv[16, 4] = 1.0
    sv[48, 4] = 1.0
    sv[:, 5] = -1.0 / 16.0
    sh["sv"] = sv
    sh["wout"] = inp["w_out"][0]
    sh["rw"] = np.ascontiguousarray(inp["router_w"][0].reshape(16, 128, 64).transpose(1, 0, 2))
    sh["rbias"] = np.ascontiguousarray(np.broadcast_to(inp["router_bias"][0][None], (128, 64)))
    sh["ew1"] = inp["expert_w1"][0]
    sh["ew3"] = inp["expert_w3"][0]
    sh["ew2"] = inp["expert_w2"][0]
    sh["sw1"] = inp["shared_w1"][0]
    sh["sw3"] = inp["shared_w3"][0]
    sh["sw2"] = inp["shared_w2"][0]
    i = np.arange(128)
    s_, t_ = i[:, None], i[None, :]
    c = np.float32(-1.0 / 16.0)
    cm = np.zeros((128, 8, 128), np.float32)
    cm[:, 0] = np.eye(128)
    cm[:, 1] = np.where(s_ > t_, c, 0)
    cm[:, 2] = np.where(s_ < t_, c, 0)
    cm[:, 3] = np.where(s_ <= t_, c, 0)
    cm[:, 4] = np.where(s_ >= t_, c, 0)
    cm[:, 5] = (s_ <= t_)
    cm[:, 6] = (s_ >= t_)
    cm[:, 7] = 1.0
    sh["cm"] = cm
    return sh


def _prep_core(inp, c):
    b, q = c // 4, c % 4
    x = inp["x"][b]
    ctx = inp["ctx"][b]
    m = {}
    m["xo"] = np.ascontiguousarray(x[q * 1024:(q + 1) * 1024])
    xh = np.zeros((512, 2048), np.float32)
    for s, g in enumerate(_halo_chunks(q)):
        if g is not None:
            xh[s * 128:(s + 1) * 128] = x[g * 128:(g + 1) * 128]
    m["xh"] = xh
    m["xc"] = np.ascontiguousarray(ctx)
    m["xg"] = np.concatenate([ctx[::-1], ctx, x[:q * 1024], x[(q + 1) * 1024:][::-1]], axis=0)
    cvec = np.stack([inp["c"][b], inp["c_ctx"]], axis=0)
    m["cT"] = np.ascontiguousarray(cvec.reshape(2, 16, 128).transpose(2, 1, 0))
    t_own = q * 1024 + np.arange(1024)
    cosO, sinO = _rope_tables(t_own // 64, t_own % 64)
    m["ropeO"] = np.ascontiguousarray(np.stack([cosO.reshape(8, 128, 128), sinO.reshape(8, 128, 128)], axis=1)
                                      .transpose(2, 0, 1, 3))
    m["ropeT"] = np.ascontiguousarray(np.stack([cosO.T, sinO.T], axis=1))
    t_lat = np.concatenate([np.arange(0, q * 1024), np.arange((q + 1) * 1024, 4096)[::-1]])
    cosG, sinG = _rope_tables(t_lat // 64, t_lat % 64)
    cosG = np.concatenate([np.ones((512, 128), np.float32), cosG], axis=0)
    sinG = np.concatenate([np.zeros((512, 128), np.float32), sinG], axis=0)
    m["ropeG"] = np.ascontiguousarray(np.stack([cosG.reshape(28, 128, 128), sinG.reshape(28, 128, 128)], axis=1)
                                      .transpose(2, 0, 1, 3))
    isf = np.concatenate([np.zeros(256), np.ones(256), np.ones(q * 1024), np.zeros((3 - q) * 1024)]).astype(np.float32)
    sel = np.zeros((64, 3584), np.float32)
    sel[0:32] = isf[None]
    sel[32:64] = 1.0 - isf[None]
    m["selG"] = sel
    fl = np.zeros((128, 8), np.float32)
    fl[:, q] = 1.0
    fl[:, 4:8] = 1.0 - fl[:, 0:4]
    m["flags"] = fl
    m["nabias"] = _na_bias(q, inp["na_rpb"][0])
    return m


def build_program(shapes):
    nc = bass.Bass("TRN2", target_bir_lowering=False)
    dr = {}
    for name, shp in shapes.items():
        dr[name] = nc.dram_tensor(name, list(shp), F32, kind="ExternalInput").ap()
    out_d = nc.dram_tensor("out", [1024, 2048], F32, kind="ExternalOutput").ap()
    modscr = nc.dram_tensor("modscr", [2, 12288], F32, kind="Internal").ap()
    x1scr = nc.dram_tensor("x1scr", [1024, 2048], F32, kind="Internal").ap()
    dbg = {}
    if DEBUG:
        dbg["mixT"] = nc.dram_tensor("dbg_mixT", [128, 16 * 1024], F32, kind="ExternalOutput").ap()
        dbg["x1"] = nc.dram_tensor("dbg_x1", [1024, 2048], F32, kind="ExternalOutput").ap()
        dbg["comb"] = nc.dram_tensor("dbg_comb", [1024, 64], F32, kind="ExternalOutput").ap()

    with ExitStack() as st:
        S = Sched(nc, st)
        AW = 47 * 1024
        arena = st.enter_context(nc.sbuf_tensor("arena", [128, AW], F32))
        top = [0]

        def alloc(words, dt=F32, parts=128):
            w = (words + 1) // 2 * 2
            assert top[0] + w <= AW, ("arena overflow", top[0], w)
            ap = arena[0:parts, top[0]:top[0] + w]
            top[0] += w
            if dt == BF16:
                ap = ap.bitcast(BF16)
            return Buf(ap)

        def abf(n, parts=128):
            return alloc((n + 1) // 2, BF16, parts)

        def release(mark):
            PHASE_MARKS.append(sum(1 for o in S.ops["dve"] if o[1] is not None))
            S.barrier()
            top[0] = mark

        PSB = []
        for i in range(8):
            PSB.append(Buf(st.enter_context(nc.psum_tensor("ps%d" % i, [128, 512], F32))[:]))

        def mm(out, lhsT, rhs, start, stop, rd, wr, sig=None):
            S.op("pe", lambda e: e.matmul(out, lhsT, rhs, start=start, stop=stop), reads=rd, writes=wr,
                 signal=bool(stop) if sig is None else sig)

        def tr(out, in_, ident, rd, wr, sig=True):
            S.op("pe", lambda e: e.transpose(out, in_, ident), reads=rd, writes=wr, signal=sig)

        def act(out, in_, func, rd, wr, **kw):
            S.op("act", lambda e: e.activation(out=out, in_=in_, func=func, **kw), reads=rd, writes=wr)

        def tt(eng, out, a, b, op, rd, wr):
            S.op(eng, lambda e: e.tensor_tensor(out, a, b, op), reads=rd, writes=wr)

        def ts(eng, out, a, s1, s2, op0, op1, rd, wr):
            if s2 is None:
                S.op(eng, lambda e: e.tensor_scalar(out, a, s1, None, op0), reads=rd, writes=wr)
            else:
                S.op(eng, lambda e: e.tensor_scalar(out, a, s1, s2, op0, op1), reads=rd, writes=wr)

        def stt(eng, out, a, sc, b, op0, op1, rd, wr):
            S.op(eng, lambda e: e.scalar_tensor_tensor(out, a, sc, b, op0, op1), reads=rd, writes=wr)

        def cp(eng, out, in_, rd, wr):
            if eng == "act":
                act(out, in_, AF.Copy, rd, wr)
            else:
                S.op(eng, lambda e: e.tensor_copy(out, in_), reads=rd, writes=wr)

        def dma(q, out, in_, rd, wr):
            return S.dma(q, lambda e: e.dma_start(out=out, in_=in_), reads=rd, writes=wr)

        def memset(eng, out, val, wr):
            S.op(eng, lambda e: e.memset(out, val), writes=wr)

        cm32 = alloc(8 * 128)
        dma("sp", cm32.ap.rearrange("p (a b) -> p a b", a=8), dr["cm"], [], [cm32.r])
        cmv = cm32.ap.rearrange("p (a b) -> p a b", a=8)
        ID32, TU, TLS, TLI, TUI = (cmv[:, k, :] for k in range(5))
        cm16 = abf(8 * 128)
        dma("pool", cm16.ap.rearrange("p (a b) -> p a b", a=8), dr["cm"], [], [cm16.r])
        cmb = cm16.ap.rearrange("p (a b) -> p a b", a=8)
        ID16, MF16, MB16, ONES16 = cmb[:, 0, :], cmb[:, 5, :], cmb[:, 6, :], cmb[:, 7, :]
        MF32, MB32 = cmv[:, 5, :], cmv[:, 6, :]
        sv = alloc(8)
        dma("sp", sv.ap, dr["sv"], [], [sv.r])
        QNG, KNG, GNG0, GNG1, DBIAS, NEG16 = (sv.ap[:, k:k + 1] for k in range(6))
        flags = alloc(8)
        dma("sp", flags.ap, dr["flags"], [], [flags.r])
        UP = alloc(512, F32, 64)
        dma("sp", UP.ap, dr["UP"], [], [UP.r])
        CR = [cm32.r, cm16.r, sv.r]
        stat = [alloc(4) for _ in range(4)]
        stat_i = [0]

        cTs = abf(32)
        m0 = top[0]
        cT = alloc(32)
        dma("sp", cT.ap.rearrange("p (a b) -> p a b", a=16), dr["cT"], [], [cT.r])
        act(cTs.ap, cT.ap, AF.Silu, [cT.r], [cTs.r])
        cTv = cTs.ap.rearrange("p (a b) -> p a b", a=16)
        modrows = alloc(4096, F32, 2)
        wm = [abf(16 * 512) for _ in range(2)]
        bm = [alloc(512, F32, 2) for _ in range(2)]
        for cb in range(8):
            W = wm[cb % 2]
            B = bm[cb % 2]
            ps = PSB[cb % 2]
            dma("pool", W.ap, dr["wmod"][cb].rearrange("p a b -> p (a b)"), [], [W.r])
            dma("sp", B.ap, dr["bmod2"][:, cb * 512:(cb + 1) * 512], [], [B.r])
            Wv = W.ap.rearrange("p (a b) -> p a b", a=16)
            for kc in range(16):
                mm(ps.ap[0:2, :], cTv[:, kc, :], Wv[:, kc, :], kc == 0, kc == 15, [cTs.r, W.r], [ps.r])
            tt("dve", modrows.ap[:, cb * 512:(cb + 1) * 512], ps.ap[0:2, :], B.ap, ALU.add, [ps.r, B.r], [modrows.r])
        ng = alloc(2048, F32, 2)
        dma("sp", ng.ap, dr["n1g2"], [], [ng.r])
        sl = modrows.ap[:, 2048:4096]
        stt("dve", sl, sl, 1.0, ng.ap, ALU.add, ALU.mult, [modrows.r, ng.r], [modrows.r])
        rms = Sched.res()
        dma("sp", modscr[:, 0:4096], modrows.ap, [modrows.r], [rms])
        release(m0)

        def load_bcast(buf, row, sec):
            dma("sp", buf.ap, modscr[row:row + 1, sec * 2048:(sec + 1) * 2048].to_broadcast([128, 2048]), [rms], [buf.r])

        def rstd_from_ssq(stb, n):
            ts("dve", stb.ap[:, 1:2], stb.ap[:, 0:1], 1.0 / n, EPS, ALU.mult, ALU.add, [stb.r], [stb.r])
            act(stb.ap[:, 1:2], stb.ap[:, 1:2], AF.Ln, [stb.r], [stb.r])
            act(stb.ap[:, 1:2], stb.ap[:, 1:2], AF.Exp, [stb.r], [stb.r], scale=-0.5)

        class HT:
            def __init__(self):
                self.xt = [alloc(2048) for _ in range(2)]
                self.junk = abf(2048)
                self.xn = [abf(2048) for _ in range(2)]
                self.i = 0

            def elem(self, src, gm, shf):
                idx = self.i
                xt = self.xt[idx % 2]
                xn = self.xn[idx % 2]
                self.i += 1
                stb = stat[stat_i[0] % 4]
                stat_i[0] += 1
                dma("sp", xt.ap, src, [], [xt.r])
                memset("dve", stb.ap, 0.0, [stb.r])
                act(self.junk.ap, xt.ap, AF.Square, [xt.r], [self.junk.r, stb.r], accum_out=stb.ap[:, 0:1])
                rstd_from_ssq(stb, 2048)
                stt("dve", xt.ap, xt.ap, stb.ap[:, 1:2], gm.ap, ALU.mult, ALU.mult, [xt.r, stb.r, gm.r], [xt.r])
                tt("dve", xn.ap, xt.ap, shf.ap, ALU.add, [xt.r, shf.r], [xn.r])
                return idx

            def trs(self, idx, dst_ap, dst_r, psA, psB):
                xn = self.xn[idx % 2]
                for half, ps in ((0, psA), (1, psB)):
                    pv = ps.ap.bitcast(BF16).rearrange("p (a b) -> p a b", a=8)
                    for k8 in range(8):
                        kc = half * 8 + k8
                        tr(pv[:, k8, :], xn.ap[:, kc * 128:(kc + 1) * 128], ID16, [xn.r, cm16.r], [ps.r], k8 == 7)
                    cp("act" if half == 0 else "dve", dst_ap[:, half * 8:(half + 1) * 8, :], pv, [ps.r], [dst_r])

            def make(self, src, gm, shf, dst_ap, dst_r, psA, psB):
                self.trs(self.elem(src, gm, shf), dst_ap, dst_r, psA, psB)

        def v3(buf, a):
            return buf.ap.rearrange("p (a b) -> p a b", a=a)

        def rope_tok(kps_ap, kps_r, cos_ap, sin_ap, tab_r, out, t2, nh):
            cosb = cos_ap.rearrange("p (o c) -> p o c", o=1).to_broadcast([128, nh, 128])
            tt("dve", out.ap.rearrange("p (h c) -> p h c", h=nh), kps_ap.rearrange("p (h c) -> p h c", h=nh), cosb, ALU.mult,
               [kps_r, tab_r], [out.r])
            s4 = sin_ap.rearrange("p (a b c) -> p a b c", a=2, b=2)
            for bb in range(2):
                for a in range(2):
                    sb = s4[:, a, bb, :].rearrange("p (o c) -> p o c", o=1).to_broadcast([128, nh, 32])
                    kin = kps_ap.rearrange("p (h a b c) -> p h a b c", h=nh, a=2, b=2)[:, :, a, 1 - bb, :]
                    tout = t2.ap.rearrange("p (h a b c) -> p h a b c", h=nh, a=2, b=2)[:, :, a, bb, :]
                    tt("dve", tout, kin, sb, ALU.mult, [kps_r, tab_r], [t2.r])
            tt("pool", out.ap, out.ap, t2.ap, ALU.add, [out.r, t2.r], [out.r])

        Sst = alloc(1024)
        Scb = alloc(1024)
        Sfs = alloc(1024)
        memset("dve", Sst.ap, 0.0, [Sst.r])
        memset("dve", Sfs.ap, 0.0, [Sfs.r])
        mA = top[0]
        gm1L, sh1L, gm1C, sh1C = (alloc(2048) for _ in range(4))
        load_bcast(gm1L, 0, 1)
        load_bcast(sh1L, 0, 0)
        load_bcast(gm1C, 1, 1)
        load_bcast(sh1C, 1, 0)
        ht = HT()
        wkv = abf(16 * 1536)
        wkvv = v3(wkv, 16)
        for blk in range(3):
            dma("pool", wkvv[:, :, blk * 512:(blk + 1) * 512], dr["wB"][blk], [], [wkv.r])
        wdn = abf(16 * 64)
        wdnv = v3(wdn, 16)
        dma("pool", wdnv, dr["wdn"], [], [wdn.r])
        hTc = [abf(16 * 128) for _ in range(2)]
        vt = [abf(1024) for _ in range(2)]
        rp = [alloc(256) for _ in range(2)]
        krb = [alloc(512) for _ in range(2)]
        t2 = alloc(512)
        Dtb = [alloc(128, F32, 64) for _ in range(2)]
        selG = alloc(3584, F32, 64)
        dma("sp", selG.ap, dr["selG"], [], [selG.r])
        e1 = alloc(512)
        sp_ = alloc(512)
        Eb = alloc(512)
        kd = abf(512)
        dec = alloc(4)

        def checkpoint(col):
            m_, nm_ = flags.ap[:, col:col + 1], flags.ap[:, 4 + col:5 + col]
            stt("dve", Sfs.ap, Sst.ap, m_, Sfs.ap, ALU.mult, ALU.add, [Sst.r, Sfs.r, flags.r], [Sfs.r])
            ts("dve", Sst.ap, Sst.ap, nm_, None, ALU.mult, None, [Sst.r, flags.r], [Sst.r])
            stt("dve", Sst.ap, Scb.ap, m_, Sst.ap, ALU.mult, ALU.add, [Scb.r, Sst.r, flags.r], [Sst.r])

        def s1_elem(j):
            xt = ht.xt[j % 2]
            xn = ht.xn[j % 2]
            stb = stat[stat_i[0] % 4]
            stat_i[0] += 1
            gm, shf = (gm1C, sh1C) if j < 4 else (gm1L, sh1L)
            dma("sp", xt.ap, dr["xg"][j * 128:(j + 1) * 128, :], [], [xt.r])
            dma("sp", v3(rp[j % 2], 2), dr["ropeG"][:, j], [], [rp[j % 2].r])
            memset("dve", stb.ap, 0.0, [stb.r])
            act(ht.junk.ap, xt.ap, AF.Square, [xt.r], [ht.junk.r, stb.r], accum_out=stb.ap[:, 0:1])
            rstd_from_ssq(stb, 2048)
            stt("dve", xt.ap, xt.ap, stb.ap[:, 1:2], gm.ap, ALU.mult, ALU.mult, [xt.r, stb.r, gm.r], [xt.r])
            tt("dve", xn.ap, xt.ap, shf.ap, ALU.add, [xt.r, shf.r], [xn.r])

        def s1_tr(j):
            xn = ht.xn[j % 2]
            H = hTc[j % 2]
            Hv = v3(H, 16)
            ps = PSB[0]
            pv = ps.ap.bitcast(BF16).rearrange("p (a b) -> p a b", a=8)
            for half in range(2):
                for k8 in range(8):
                    kc = half * 8 + k8
                    tr(pv[:, k8, :], xn.ap[:, kc * 128:(kc + 1) * 128], ID16, [xn.r, cm16.r], [ps.r], k8 == 7)
                cp("act" if half == 0 else "dve", Hv[:, half * 8:(half + 1) * 8, :], pv, [ps.r], [H.r])

        def s3_z(j):
            Dt = Dtb[j % 2]
            mm(PSB[6].ap, Dt.ap, UP.ap, True, True, [Dt.r, UP.r], [PSB[6].r])
            act(e1.ap, PSB[6].ap, AF.Exp, [PSB[6].r], [e1.r], scale=-1.0)
            act(sp_.ap, e1.ap, AF.Ln, [e1.r], [sp_.r], bias=1.0, scale=1.0)

        def s3_rest(j):
            mm(PSB[6].ap, TU, sp_.ap, True, True, [sp_.r, cm32.r], [PSB[6].r])
            for h in range(4):
                mm(PSB[5].ap[:, 128 + h:129 + h], sp_.ap[:, h * 128:(h + 1) * 128], NEG16, True, True, [sp_.r, sv.r], [PSB[5].r])
            act(Eb.ap, PSB[6].ap, AF.Exp, [PSB[6].r], [Eb.r])
            act(dec.ap, PSB[5].ap[:, 128:132], AF.Exp, [PSB[5].r], [dec.r])
            tt("dve", kd.ap, krb[j % 2].ap, Eb.ap, ALU.mult, [krb[j % 2].r, Eb.r], [kd.r])

        def s3_state(j):
            V_ = vt[j % 2]
            for h in range(4):
                ps = PSB[1] if h < 2 else PSB[7]
                mm(ps.ap[:, (h % 2) * 256:(h % 2 + 1) * 256], kd.ap[:, h * 128:(h + 1) * 128], V_.ap[:, h * 256:(h + 1) * 256],
                   True, True, [kd.r, V_.r], [ps.r])
            for h in range(4):
                ps = PSB[1] if h < 2 else PSB[7]
                stt("dve", Sst.ap[:, h * 256:(h + 1) * 256], Sst.ap[:, h * 256:(h + 1) * 256], dec.ap[:, h:h + 1],
                    ps.ap[:, (h % 2) * 256:(h % 2 + 1) * 256], ALU.mult, ALU.add, [Sst.r, dec.r, ps.r], [Sst.r])
            if j == 1:
                cp("dve", Scb.ap, Sst.ap, [Sst.r], [Scb.r])
                memset("dve", Sst.ap, 0.0, [Sst.r])
            if j in (3, 11, 19, 27):
                checkpoint((j - 3) // 8)

        Wbg = abf(16 * 256)
        bbg = [alloc(256, F32, 2) for _ in range(2)]
        nbg = [alloc(256, F32, 2) for _ in range(2)]
        rbg = [alloc(256, F32, 2) for _ in range(2)]
        bg_i = [0]

        def adaln_bg():
            hb = bg_i[0]
            if hb >= 32:
                return
            bg_i[0] += 1
            c0 = 4096 + hb * 256
            blk, half = c0 // 512, (c0 % 512) // 256
            B_, N_, R_ = bbg[hb % 2], nbg[hb % 2], rbg[hb % 2]
            Wv_ = v3(Wbg, 16)
            dma("pool", Wv_, dr["wmod"][blk][:, :, half * 256:(half + 1) * 256], [], [Wbg.r])
            dma("sp", B_.ap, dr["bmod2"][:, c0:c0 + 256], [], [B_.r])
            reg = PSB[5].ap[0:2, 256:512]
            for kc in range(16):
                mm(reg, cTv[:, kc, :], Wv_[:, kc, :], kc == 0, kc == 15, [cTs.r, Wbg.r], [PSB[5].r])
            tt("dve", R_.ap, reg, B_.ap, ALU.add, [PSB[5].r, B_.r], [R_.r])
            if 8192 <= c0 < 10240:
                dma("sp", N_.ap, dr["n2g2"][:, c0 - 8192:c0 - 8192 + 256], [], [N_.r])
                stt("dve", R_.ap, R_.ap, 1.0, N_.ap, ALU.add, ALU.mult, [R_.r, N_.r], [R_.r])
            dma("sp", modscr[:, c0:c0 + 256], R_.ap, [R_.r], [rms])

        s1_elem(0)
        s1_tr(0)
        for j in range(29):
            adaln_bg()
            if j < 3:
                adaln_bg()
            if j < 28:
                H = hTc[j % 2]
                Hv = v3(H, 16)
                V_, R_, KR, Dt = vt[j % 2], rp[j % 2], krb[j % 2], Dtb[j % 2]
                for kc in range(16):
                    mm(PSB[2].ap, Hv[:, kc, :], wkvv[:, kc, 0:512], kc == 0, kc == 15, [H.r, wkv.r], [PSB[2].r])
                rope_tok(PSB[2].ap, PSB[2].r, R_.ap[:, 0:128], R_.ap[:, 128:256], R_.r, KR, t2, 4)
            if j + 1 < 28:
                s1_elem(j + 1)
            if j >= 1:
                s3_z(j - 1)
            if j < 28:
                for kc in range(16):
                    mm(PSB[3].ap, Hv[:, kc, :], wkvv[:, kc, 512:1024], kc == 0, kc == 15, [H.r, wkv.r], [PSB[3].r])
                cp("act", V_.ap[:, 0:512], PSB[3].ap, [PSB[3].r], [V_.r])
            if j >= 1:
                s3_rest(j - 1)
            if j < 28:
                for kc in range(16):
                    mm(PSB[4].ap, Hv[:, kc, :], wkvv[:, kc, 1024:1536], kc == 0, kc == 15, [H.r, wkv.r], [PSB[4].r])
                cp("act", V_.ap[:, 512:1024], PSB[4].ap, [PSB[4].r], [V_.r])
            if j >= 1:
                s3_state(j - 1)
            if j < 28:
                for kc in range(16):
                    mm(PSB[5].ap[0:64, 0:128], wdnv[:, kc, :], Hv[:, kc, :], kc == 0, kc == 15, [H.r, wdn.r], [PSB[5].r])
                stt("dve", Dt.ap, PSB[5].ap[0:64, 0:128], DBIAS[0:64, :], selG.ap[:, j * 128:(j + 1) * 128], ALU.add, ALU.mult,
                    [PSB[5].r, sv.r, selG.r], [Dt.r])
            if j + 1 < 28:
                s1_tr(j + 1)
        release(mA)

        mixT = abf(16 * 1024)
        mixv = v3(mixT, 16)
        hTown = abf(16 * 1024)
        hTo = v3(hTown, 16)
        hTr = [Sched.res() for _ in range(8)]
        hT6 = abf(16 * 768)
        hT6v = v3(hT6, 16)
        hT6r = [Sched.res() for _ in range(6)]
        mB = top[0]
        gm1L, sh1L, gm1C, sh1C = (alloc(2048) for _ in range(4))
        load_bcast(gm1L, 0, 1)
        load_bcast(sh1L, 0, 0)
        load_bcast(gm1C, 1, 1)
        load_bcast(sh1C, 1, 0)
        ht = HT()

        def b_args(j):
            if j < 8:
                return (dr["xo"][j * 128:(j + 1) * 128, :], gm1L, sh1L, hTo[:, :, j * 128:(j + 1) * 128], hTr[j])
            jj = j - 8
            src = dr["xh"][jj * 128:(jj + 1) * 128, :] if jj < 4 else dr["xc"][(jj - 4) * 128:(jj - 3) * 128, :]
            return (src, gm1L if jj < 4 else gm1C, sh1L if jj < 4 else sh1C, hT6v[:, :, jj * 128:(jj + 1) * 128], hT6r[jj])

        ids = {}
        a0 = b_args(0)
        ids[0] = ht.elem(a0[0], a0[1], a0[2])
        for j in range(14):
            if j + 1 < 14:
                a1 = b_args(j + 1)
                ids[j + 1] = ht.elem(a1[0], a1[1], a1[2])
            aj = b_args(j)
            ht.trs(ids[j], aj[3], aj[4], PSB[(2 * j) % 8], PSB[(2 * j + 1) % 8])
        release(mB)

        def load_w(buf, idx):
            dma("pool", buf.ap, dr["wA"][idx].rearrange("p a b -> p (a b)"), [], [buf.r])
            return v3(buf, 16)

        qTh = [abf(1024) for _ in range(2)]
        kTh = [abf(1792) for _ in range(2)]
        vh = [abf(14 * 128) for _ in range(2)]
        wq_ = [abf(16 * 128) for _ in range(2)]
        wk_ = [abf(16 * 128) for _ in range(2)]
        wv_ = [abf(16 * 128) for _ in range(2)]
        sq16 = [abf(512) for _ in range(2)]
        rs32 = [alloc(512) for _ in range(2)]
        bt = [alloc(640) for _ in range(2)]
        sA = [alloc(640) for _ in range(2)]
        PT = [abf(896) for _ in range(2)]
        rden = [alloc(128) for _ in range(2)]
        it = [0]
        scale = 128.0 ** -0.5

        def qk_norm_unit(Wv, W, hsrc, hres, n, dst_ap, dst_r, gvec, pa, pb):
            k = it[0]
            it[0] += 1
            sq, rs = sq16[k % 2], rs32[k % 2]
            for kc in range(16):
                mm(pa.ap[:, 0:n], Wv[:, kc, :], hsrc[:, kc, :], kc == 0, kc == 15, [W.r] + hres, [pa.r])
            act(sq.ap[:, 0:n], pa.ap[:, 0:n], AF.Square, [pa.r], [sq.r])
            mm(pb.ap[:, 0:n], ONES16, sq.ap[:, 0:n], True, True, [sq.r, cm16.r], [pb.r])
            act(rs.ap[:, 0:n], pb.ap[:, 0:n], AF.Ln, [pb.r], [rs.r], scale=1.0 / 128, bias=EPS)
            act(rs.ap[:, 0:n], rs.ap[:, 0:n], AF.Exp, [rs.r], [rs.r], scale=-0.5)
            stt("dve", dst_ap, pa.ap[:, 0:n], gvec, rs.ap[:, 0:n], ALU.mult, ALU.mult, [pa.r, rs.r, sv.r], [dst_r])

        def na_proj(h):
            hp = h % 2
            Q_, K_, V_ = qTh[hp], kTh[hp], vh[hp]
            Wq, Wk, Wvv_ = wq_[hp], wk_[hp], wv_[hp]
            Wqv, Wkv, Wvv = load_w(Wq, h), load_w(Wk, 8 + h), load_w(Wvv_, 41 + h)
            for g in range(2):
                qk_norm_unit(Wqv, Wq, hTo[:, :, g * 512:(g + 1) * 512], hTr[g * 4:(g + 1) * 4], 512,
                             Q_.ap[:, g * 512:(g + 1) * 512], Q_.r, QNG, PSB[0], PSB[1])
                yield
            for g in range(2):
                qk_norm_unit(Wkv, Wk, hTo[:, :, g * 512:(g + 1) * 512], hTr[g * 4:(g + 1) * 4], 512,
                             K_.ap[:, g * 512:(g + 1) * 512], K_.r, KNG, PSB[0], PSB[1])
                yield
            qk_norm_unit(Wkv, Wk, hT6v[:, :, 0:512], hT6r[0:4], 512, K_.ap[:, 1024:1536], K_.r, KNG, PSB[0], PSB[1])
            yield
            qk_norm_unit(Wkv, Wk, hT6v[:, :, 512:768], hT6r[4:6], 256, K_.ap[:, 1536:1792], K_.r, KNG, PSB[0], PSB[1])
            yield
            for c4 in range(4):
                ps = PSB[c4 % 2]
                chs = list(range(c4 * 4, min(c4 * 4 + 4, 14)))
                for jj, ch in enumerate(chs):
                    if ch < 8:
                        src, sres = hTo[:, :, ch * 128:(ch + 1) * 128], [hTr[ch]]
                    else:
                        src, sres = hT6v[:, :, (ch - 8) * 128:(ch - 7) * 128], [hT6r[ch - 8]]
                    for kc in range(16):
                        mm(ps.ap[:, jj * 128:(jj + 1) * 128], src[:, kc, :], Wvv[:, kc, :], kc == 0, kc == 15, [Wvv_.r] + sres, [ps.r])
                cp("act", V_.ap[:, chs[0] * 128:(chs[-1] + 1) * 128], ps.ap[:, 0:len(chs) * 128], [ps.r], [V_.r])
                yield

        def na_chunks(i):
            chunks = []
            for j in range(5):
                sl = i - 2 + j
                chunks.append(sl if 0 <= sl <= 7 else {-2: 8, -1: 9, 8: 10, 9: 11}[sl])
            return chunks + [12, 13]

        def na_att(h):
            hp = h % 2
            Q_, K_, V_ = qTh[hp], kTh[hp], vh[hp]
            Vv = v3(V_, 14)

            def stage1(i):
                par = i % 2
                bA, bB = PSB[2 + 2 * par], PSB[3 + 2 * par]
                B_, SA_, PT_ = bt[par], sA[par], PT[par]
                dma("sp", B_.ap, dr["nabias"][h, i], [], [B_.r])
                qs = Q_.ap[:, i * 128:(i + 1) * 128]
                for j, ch in enumerate(na_chunks(i)):
                    dst = bA.ap[:, j * 128:(j + 1) * 128] if j < 4 else bB.ap[:, (j - 4) * 128:(j - 3) * 128]
                    mm(dst, K_.ap[:, ch * 128:(ch + 1) * 128], qs, True, True, [K_.r, Q_.r], [bA.r if j < 4 else bB.r])
                stt("dve", SA_.ap[:, 0:512], bA.ap, scale, B_.ap[:, 0:512], ALU.mult, ALU.add, [bA.r, B_.r], [SA_.r])
                stt("dve", SA_.ap[:, 512:640], bB.ap[:, 0:128], scale, B_.ap[:, 512:640], ALU.mult, ALU.add, [bB.r, B_.r], [SA_.r])
                act(PT_.ap[:, 0:640], SA_.ap, AF.Exp, [SA_.r], [PT_.r])
                act(PT_.ap[:, 640:896], bB.ap[:, 128:384], AF.Exp, [bB.r], [PT_.r], scale=scale)

            def stage2(i):
                par = i % 2
                bC, bD = PSB[6], PSB[7]
                PT_, RD_ = PT[par], rden[par]
                chunks = na_chunks(i)
                for j, ch in enumerate(chunks):
                    mm(bC.ap[:, 0:128], Vv[:, ch, :], PT_.ap[:, j * 128:(j + 1) * 128], j == 0, j == 6, [V_.r, PT_.r], [bC.r])
                for j, ch in enumerate(chunks):
                    mm(bD.ap[:, 0:128], ONES16, PT_.ap[:, j * 128:(j + 1) * 128], j == 0, j == 6, [cm16.r, PT_.r], [bD.r])
                S.op("dve", lambda e, o=RD_.ap, a=bD.ap[:, 0:128]: e.reciprocal(o, a), reads=[bD.r], writes=[RD_.r])
                tt("dve", mixv[:, h, i * 128:(i + 1) * 128], bC.ap[:, 0:128], RD_.ap, ALU.mult, [bC.r, RD_.r], [mixT.r])

            stage1(0)
            yield
            for i in range(8):
                if i + 1 < 8:
                    stage1(i + 1)
                    yield
                stage2(i)
                yield

        def interleave(g1, g2):
            a1, a2 = True, True
            while a1 or a2:
                if a1:
                    try:
                        next(g1)
                    except StopIteration:
                        a1 = False
                if a2:
                    try:
                        next(g2)
                    except StopIteration:
                        a2 = False

        interleave(na_proj(0), iter(()))
        for h in range(8):
            interleave(na_att(h), na_proj(h + 1) if h + 1 < 8 else iter(()))
        release(mB - (16 * 768) // 2)

        wdn = abf(16 * 64)
        wdnv = v3(wdn, 16)
        dma("pool", wdnv, dr["wdn"], [], [wdn.r])
        DtO = alloc(1024, F32, 64)
        for g in range(2):
            pa = PSB[g]
            hs = hTo[:, :, g * 512:(g + 1) * 512]
            for kc in range(16):
                mm(pa.ap[0:64, :], wdnv[:, kc, :], hs[:, kc, :], kc == 0, kc == 15, [wdn.r] + hTr[g * 4:(g + 1) * 4], [pa.r])
            ts("dve", DtO.ap[:, g * 512:(g + 1) * 512], pa.ap[0:64, :], DBIAS[0:64, :], None, ALU.add, None, [pa.r, sv.r], [DtO.r])
        ropeT = alloc(2048)
        dma("sp", v3(ropeT, 2), dr["ropeT"], [], [ropeT.r])
        rTv = v3(ropeT, 2)
        ropeO = alloc(8 * 256)
        dma("sp", ropeO.ap.rearrange("p (a b c) -> p a b c", a=8, b=2), dr["ropeO"], [], [ropeO.r])
        rOv = v3(ropeO, 8)
        WG = [abf(16 * 128) for _ in range(8)]
        qr = abf(1024)
        krf = abf(1024)
        gate = abf(2 * 1024)
        gav = v3(gate, 2)
        vown = abf(8 * 256)
        vov = v3(vown, 8)
        kdF, kdB = abf(8 * 128), abf(8 * 128)
        QF, KF, QB, KB = (abf(8 * 128) for _ in range(4))
        SfT, SbT = abf(8 * 256), abf(8 * 256)
        decO = alloc(16)
        tmpa = [alloc(512)] * 2
        tmpb = [alloc(512)] * 2
        kr1b = [alloc(128) for _ in range(2)]
        t21 = alloc(128)
        e11, sp1, Eb1, epos, eneg = ([alloc(128) for _ in range(2)] for _ in range(5))
        AFb = [abf(128) for _ in range(2)]
        ABb = [abf(128) for _ in range(2)]
        oT = [alloc(256) for _ in range(2)]
        osq = [abf(256) for _ in range(2)]
        rsg = [alloc(128) for _ in range(2)]
        qscale = 128.0 ** -0.5
        kk = 0
        for h in range(4):
            Wq, Wqs, Wk, Wks, Wg0, Wg1, Wv0, Wv1 = WG
            Wqv, Wqsv, Wkv, Wksv = load_w(Wq, 16 + h), load_w(Wqs, 20 + h), load_w(Wk, 24 + h), load_w(Wks, 28 + h)
            Wg0v, Wg1v = load_w(Wg0, 32 + 2 * h), load_w(Wg1, 33 + 2 * h)
            Wv0v, Wv1v = load_w(Wv0, 49 + 2 * h), load_w(Wv1, 50 + 2 * h)
            for (Wa, Wav, Wb, Wbv, dst) in ((Wq, Wqv, Wqs, Wqsv, qr), (Wk, Wkv, Wks, Wksv, krf)):
                for g in range(2):
                    pa, pb = PSB[(2 * kk) % 8], PSB[(2 * kk + 1) % 8]
                    ta, tb = tmpa[kk % 2], tmpb[kk % 2]
                    kk += 1
                    hs = hTo[:, :, g * 512:(g + 1) * 512]
                    for kc in range(16):
                        mm(pa.ap, Wav[:, kc, :], hs[:, kc, :], kc == 0, kc == 15, [Wa.r] + hTr[g * 4:(g + 1) * 4], [pa.r])
                    for kc in range(16):
                        mm(pb.ap, Wbv[:, kc, :], hs[:, kc, :], kc == 0, kc == 15, [Wb.r] + hTr[g * 4:(g + 1) * 4], [pb.r])
                    tt("dve", ta.ap, pa.ap, rTv[:, 0, g * 512:(g + 1) * 512], ALU.mult, [pa.r, ropeT.r], [ta.r])
                    tt("dve", tb.ap, pb.ap, rTv[:, 1, g * 512:(g + 1) * 512], ALU.mult, [pb.r, ropeT.r], [tb.r])
                    tt("pool", dst.ap[:, g * 512:(g + 1) * 512], ta.ap, tb.ap, ALU.add, [ta.r, tb.r], [dst.r])
            for vc, (Wg, Wgv) in enumerate(((Wg0, Wg0v), (Wg1, Wg1v))):
                for g in range(2):
                    pa = PSB[kk % 8]
                    kk += 1
                    hs = hTo[:, :, g * 512:(g + 1) * 512]
                    for kc in range(16):
                        mm(pa.ap, Wgv[:, kc, :], hs[:, kc, :], kc == 0, kc == 15, [Wg.r] + hTr[g * 4:(g + 1) * 4], [pa.r])
                    act(gav[:, vc, g * 512:(g + 1) * 512], pa.ap, AF.Silu, [pa.r], [gate.r])
            def d_pstage(i):
                tok = slice(i * 128, (i + 1) * 128)
                Hs = hTo[:, :, tok]
                pk, pv = PSB[0], PSB[1]
                for kc in range(16):
                    mm(pk.ap[:, 0:128], Hs[:, kc, :], Wkv[:, kc, :], kc == 0, kc == 15, [hTr[i], Wk.r], [pk.r])
                for vc, (Wv_, Wvv_) in enumerate(((Wv0, Wv0v), (Wv1, Wv1v))):
                    for kc in range(16):
                        mm(pv.ap[:, vc * 128:(vc + 1) * 128], Hs[:, kc, :], Wvv_[:, kc, :], kc == 0, kc == 15, [hTr[i], Wv_.r], [pv.r])
                cp("act", vov[:, i, :], pv.ap[:, 0:256], [pv.r], [vown.r])
                rope_tok(pk.ap[:, 0:128], pk.r, rOv[:, i, 0:128], rOv[:, i, 128:256], ropeO.r, kr1b[i % 2], t21, 1)

            def d_chains(i):
                tok = slice(i * 128, (i + 1) * 128)
                kr1 = kr1b[i % 2]
                dirs = ((0, TU, TLI, kdF, QF, KF), (32, TLS, TUI, kdB, QB, KB))
                pz, prs, pc = (PSB[2], PSB[3]), (PSB[4], PSB[5]), (PSB[6], PSB[7])
                for d_, (lo, TR, TC, kdb, Qb, Kb) in enumerate(dirs):
                    mm(pz[d_].ap[:, 0:128], DtO.ap[lo:lo + 32, tok], UP.ap[lo:lo + 32, h * 128:(h + 1) * 128], True, True,
                       [DtO.r, UP.r], [pz[d_].r])
                for d_ in range(2):
                    act(e11[d_].ap, pz[d_].ap[:, 0:128], AF.Exp, [pz[d_].r], [e11[d_].r], scale=-1.0)
                for d_ in range(2):
                    act(sp1[d_].ap, e11[d_].ap, AF.Ln, [e11[d_].r], [sp1[d_].r], bias=1.0, scale=1.0)
                for d_, (lo, TR, TC, kdb, Qb, Kb) in enumerate(dirs):
                    mm(prs[d_].ap[:, 0:128], TR, sp1[d_].ap, True, True, [sp1[d_].r, cm32.r], [prs[d_].r])
                    mm(pz[d_].ap[:, 128:129], sp1[d_].ap, NEG16, True, True, [sp1[d_].r, sv.r], [pz[d_].r])
                    mm(pc[d_].ap[:, 0:128], sp1[d_].ap, TC, True, True, [sp1[d_].r, cm32.r], [pc[d_].r])
                for d_ in range(2):
                    act(Eb1[d_].ap, prs[d_].ap[:, 0:128], AF.Exp, [prs[d_].r], [Eb1[d_].r])
                    act(decO.ap[:, d_ * 8 + i:d_ * 8 + i + 1], pz[d_].ap[:, 128:129], AF.Exp, [pz[d_].r], [decO.r])
                    act(epos[d_].ap, pc[d_].ap[:, 0:128], AF.Exp, [pc[d_].r], [epos[d_].r])
                    act(eneg[d_].ap, pc[d_].ap[:, 0:128], AF.Exp, [pc[d_].r], [eneg[d_].r], scale=-1.0)
                for d_, (lo, TR, TC, kdb, Qb, Kb) in enumerate(dirs):
                    tt("dve", kdb.ap[:, tok], kr1.ap, Eb1[d_].ap, ALU.mult, [kr1.r, Eb1[d_].r], [kdb.r])
                    stt("dve", Qb.ap[:, tok], qr.ap[:, tok], qscale, epos[d_].ap, ALU.mult, ALU.mult, [qr.r, epos[d_].r], [Qb.r])
                    tt("dve", Kb.ap[:, tok], krf.ap[:, tok], eneg[d_].ap, ALU.mult, [krf.r, eneg[d_].r], [Kb.r])

            d_pstage(0)
            for i in range(8):
                if i + 1 < 8:
                    d_pstage(i + 1)
                d_chains(i)
            Sf_h, Sb_h = Sfs.ap[:, h * 256:(h + 1) * 256], Sst.ap[:, h * 256:(h + 1) * 256]
            for (order, Sx, Sx_r, SxT, kdb, dcol, ps) in ((range(8), Sf_h, Sfs.r, SfT, kdF, 0, PSB[6]),
                                                       (range(7, -1, -1), Sb_h, Sst.r, SbT, kdB, 8, PSB[7])):
                for i in order:
                    cp("act", SxT.ap[:, i * 256:(i + 1) * 256], Sx, [Sx_r], [SxT.r])
                    mm(ps.ap[:, 0:256], kdb.ap[:, i * 128:(i + 1) * 128], vov[:, i, :], True, True, [kdb.r, vown.r], [ps.r])
                    stt("dve", Sx, Sx, decO.ap[:, dcol + i:dcol + i + 1], ps.ap[:, 0:256], ALU.mult, ALU.add, [Sx_r, decO.r, ps.r], [Sx_r])
            for i in range(8):
                par = i % 2
                tok = slice(i * 128, (i + 1) * 128)
                A_f, A_b, O_, Q_, R_ = AFb[par], ABb[par], oT[par], osq[par], rsg[par]
                pF, pB, pO, pS = (PSB[par * 4 + k_] for k_ in range(4))
                mm(pF.ap[:, 0:128], KF.ap[:, tok], QF.ap[:, tok], True, True, [KF.r, QF.r], [pF.r])
                mm(pB.ap[:, 0:128], KB.ap[:, tok], QB.ap[:, tok], True, True, [KB.r, QB.r], [pB.r])
                tt("dve", A_f.ap, pF.ap[:, 0:128], MF32, ALU.mult, [pF.r, cm32.r], [A_f.r])
                tt("dve", A_b.ap, pB.ap[:, 0:128], MB32, ALU.mult, [pB.r, cm32.r], [A_b.r])
                for vc in range(2):
                    reg = pO.ap[:, vc * 128:(vc + 1) * 128]
                    vs = slice(i * 256 + vc * 128, i * 256 + vc * 128 + 128)
                    mm(reg, vov[:, i, vc * 128:(vc + 1) * 128], A_f.ap, True, False, [vown.r, A_f.r], [pO.r])
                    mm(reg, vov[:, i, vc * 128:(vc + 1) * 128], A_b.ap, False, False, [vown.r, A_b.r], [pO.r])
                    mm(reg, SfT.ap[:, vs], QF.ap[:, tok], False, False, [SfT.r, QF.r], [pO.r])
                    mm(reg, SbT.ap[:, vs], QB.ap[:, tok], False, True, [SbT.r, QB.r], [pO.r])
                cp("act", O_.ap, pO.ap[:, 0:256], [pO.r], [O_.r])
                act(Q_.ap, O_.ap, AF.Square, [O_.r], [Q_.r])
                for vc in range(2):
                    mm(pS.ap[:, 0:128], ONES16, Q_.ap[:, vc * 128:(vc + 1) * 128], vc == 0, vc == 1, [Q_.r, cm16.r], [pS.r])
                act(R_.ap, pS.ap[:, 0:128], AF.Ln, [pS.r], [R_.r], scale=1.0 / 256, bias=EPS)
                act(R_.ap, R_.ap, AF.Exp, [R_.r], [R_.r], scale=-0.5)
                for vc, gv in ((0, GNG0), (1, GNG1)):
                    o_ = O_.ap[:, vc * 128:(vc + 1) * 128]
                    stt("dve", o_, o_, gv, R_.ap, ALU.mult, ALU.mult, [O_.r, R_.r, sv.r], [O_.r])
                    tt("dve", mixv[:, 8 + h * 2 + vc, tok], o_, gav[:, vc, tok], ALU.mult, [O_.r, gate.r], [mixT.r])
        if DEBUG:
            release(mB - (16 * 768) // 2)
            d32 = alloc(16 * 1024)
            cp("dve", d32.ap, mixT.ap, [mixT.r], [d32.r])
            dma("sp", dbg["mixT"], d32.ap, [d32.r], [])
        mH = mB - (16 * 768) // 2 - (16 * 1024) // 2
        release(mH)

        x1 = alloc(8 * 2048)
        x1v = v3(x1, 8)
        x1r = [Sched.res() for _ in range(8)]
        for i in range(8):
            dma("sp", x1v[:, i, :], dr["xo"][i * 128:(i + 1) * 128, :], [], [x1r[i]])
        g1t, gm2t, sh2t = (alloc(2048) for _ in range(3))
        load_bcast(g1t, 0, 2)
        load_bcast(gm2t, 0, 4)
        load_bcast(sh2t, 0, 3)
        mE = top[0]
        wo = [abf(16 * 512) for _ in range(2)]
        tw = [alloc(512) for _ in range(2)]
        k = 0
        for db in range(4):
            W = wo[db % 2]
            Wv = v3(W, 16)
            dma("pool", Wv, dr["wout"].rearrange("(c p) d -> p c d", p=128)[:, :, db * 512:(db + 1) * 512], [], [W.r])
            for i in range(8):
                ps = PSB[k % 8]
                T_ = tw[k % 2]
                k += 1
                for cc in range(16):
                    mm(ps.ap, mixv[:, cc, i * 128:(i + 1) * 128], Wv[:, cc, :], cc == 0, cc == 15, [mixT.r, W.r], [ps.r])
                tt("dve", T_.ap, ps.ap, g1t.ap[:, db * 512:(db + 1) * 512], ALU.mult, [ps.r, g1t.r], [T_.r])
                tt("pool", x1v[:, i, db * 512:(db + 1) * 512], x1v[:, i, db * 512:(db + 1) * 512], T_.ap, ALU.add,
                   [T_.r, x1r[i]], [x1r[i]])
        x1s_r = Sched.res()
        for i in range(8):
            dma("sp", x1scr[i * 128:(i + 1) * 128, :], x1v[:, i, :], [x1r[i]], [x1s_r])
            if DEBUG:
                dma("sp", dbg["x1"][i * 128:(i + 1) * 128, :], x1v[:, i, :], [x1r[i]], [])
        release(mE)

        hmT, hmv = mixT, mixv
        combT = abf(1024, 64)
        rw = alloc(16 * 64)
        dma("sp", v3(rw, 16), dr["rw"], [], [rw.r])
        rwv = v3(rw, 16)
        rbias = alloc(64)
        dma("sp", rbias.ap, dr["rbias"], [], [rbias.r])
        hm32 = [alloc(2048) for _ in range(2)]
        junk = abf(2048)
        hmT32b = [alloc(16 * 128) for _ in range(2)]
        sc = alloc(64)
        bi = alloc(64)
        b2 = alloc(64)
        eq = alloc(64)
        m1_, m2_, gs, g8, gmk, m8, ssum = (alloc(8) for _ in range(7))
        comb = alloc(64)

        def f_s1(i):
            tok = slice(i * 128, (i + 1) * 128)
            Hm = hm32[i % 2]
            hmT32 = hmT32b[i % 2]
            h32v = v3(hmT32, 16)
            stb = stat[stat_i[0] % 4]
            stat_i[0] += 1
            memset("dve", stb.ap, 0.0, [stb.r])
            act(junk.ap, x1v[:, i, :], AF.Square, [x1r[i]], [junk.r, stb.r], accum_out=stb.ap[:, 0:1])
            rstd_from_ssq(stb, 2048)
            stt("dve", Hm.ap, x1v[:, i, :], stb.ap[:, 1:2], gm2t.ap, ALU.mult, ALU.mult, [x1r[i], stb.r, gm2t.r], [Hm.r])
            tt("pool", Hm.ap, Hm.ap, sh2t.ap, ALU.add, [Hm.r, sh2t.r], [Hm.r])
            for g4 in range(4):
                ps = PSB[g4]
                for k4 in range(4):
                    kc = g4 * 4 + k4
                    tr(ps.ap[:, k4 * 128:(k4 + 1) * 128], Hm.ap[:, kc * 128:(kc + 1) * 128], ID32, [Hm.r, cm32.r], [ps.r], k4 == 3)
                cp("act", h32v[:, g4 * 4:(g4 + 1) * 4, :], ps.ap.rearrange("p (a b) -> p a b", a=4), [ps.r], [hmT32.r])
            cp("dve", hmv[:, :, tok], h32v, [hmT32.r], [hmT.r])

        def f_s2(i):
            tok = slice(i * 128, (i + 1) * 128)
            hmT32 = hmT32b[i % 2]
            h32v = v3(hmT32, 16)
            ps = PSB[4 + i % 2]
            for kc in range(16):
                mm(ps.ap[:, 0:64], h32v[:, kc, :], rwv[:, kc, :], kc == 0, kc == 15, [hmT32.r, rw.r], [ps.r])
            act(sc.ap, ps.ap[:, 0:64], AF.Sigmoid, [ps.r], [sc.r])
            tt("dve", bi.ap, sc.ap, rbias.ap, ALU.add, [sc.r, rbias.r], [bi.r])
            bi3 = bi.ap.rearrange("p (g e) -> p g e", g=8)
            S.op("dve", lambda e, o=m1_.ap, a=bi3: e.tensor_reduce(o, a, AX.X, ALU.max), reads=[bi.r], writes=[m1_.r])
            tt("dve", eq.ap.rearrange("p (g e) -> p g e", g=8), bi3, m1_.ap.rearrange("p (g o) -> p g o", o=1).to_broadcast([128, 8, 8]),
               ALU.is_equal, [bi.r, m1_.r], [eq.r])
            stt("dve", b2.ap, eq.ap, -1.0e4, bi.ap, ALU.mult, ALU.add, [eq.r, bi.r], [b2.r])
            S.op("dve", lambda e, o=m2_.ap, a=b2.ap.rearrange("p (g e) -> p g e", g=8): e.tensor_reduce(o, a, AX.X, ALU.max),
                 reads=[b2.r], writes=[m2_.r])
            tt("dve", gs.ap, m1_.ap, m2_.ap, ALU.add, [m1_.r, m2_.r], [gs.r])
            S.op("dve", lambda e, o=g8.ap, a=gs.ap: e.max(out=o, in_=a), reads=[gs.r], writes=[g8.r])
            ts("dve", gmk.ap, gs.ap, g8.ap[:, 3:4], None, ALU.is_ge, None, [gs.r, g8.r], [gmk.r])
            ts("dve", gmk.ap, gmk.ap, -1.0, 1.0e4, ALU.add, ALU.mult, [gmk.r], [gmk.r])
            tt("dve", b2.ap.rearrange("p (g e) -> p g e", g=8), bi3, gmk.ap.rearrange("p (g o) -> p g o", o=1).to_broadcast([128, 8, 8]),
               ALU.add, [bi.r, gmk.r], [b2.r])
            S.op("dve", lambda e, o=m8.ap, a=b2.ap: e.max(out=o, in_=a), reads=[b2.r], writes=[m8.r])
            ts("dve", eq.ap, b2.ap, m8.ap[:, 5:6], None, ALU.is_ge, None, [b2.r, m8.r], [eq.r])
            tt("dve", eq.ap, eq.ap, sc.ap, ALU.mult, [eq.r, sc.r], [eq.r])
            S.op("dve", lambda e, o=ssum.ap[:, 0:1], a=eq.ap: e.tensor_reduce(o, a, AX.X, ALU.add), reads=[eq.r], writes=[ssum.r])
            S.op("dve", lambda e, o=ssum.ap[:, 1:2], a=ssum.ap[:, 0:1]: e.reciprocal(o, a), reads=[ssum.r], writes=[ssum.r])
            ts("dve", comb.ap, eq.ap, ssum.ap[:, 1:2], 2.5, ALU.mult, ALU.mult, [eq.r, ssum.r], [comb.r])
            if DEBUG:
                dma("sp", dbg["comb"][tok, :], comb.ap, [comb.r], [])
            tr(PSB[6].ap[0:64, 0:128], comb.ap, ID32, [comb.r, cm32.r], [PSB[6].r])
            cp("act", combT.ap[:, tok], PSB[6].ap[0:64, 0:128], [PSB[6].r], [combT.r])

        f_s1(0)
        for i in range(8):
            if i + 1 < 8:
                f_s1(i + 1)
            f_s2(i)
        release(mH)
        combT2 = abf(1024, 64)
        cp("dve", combT2.ap, combT.ap, [combT.r], [combT2.r])
        S.barrier()
        combT = combT2

        yacc = alloc(16 * 1024)
        yv = v3(yacc, 16)
        memset("pool", yacc.ap, 0.0, [yacc.r])
        g2T = alloc(16)
        g2src = modscr[0:1, 5 * 2048:6 * 2048].rearrange("o (c p) -> p (o c)", p=128)
        S.dma("sp", lambda e: e.dma_start(out=g2T.ap, in_=g2src, allow_slow_non_contiguous=True), reads=[rms], writes=[g2T.r])
        NR = 2
        W1r = [abf(16 * 256) for _ in range(NR)]
        W3r = [abf(16 * 256) for _ in range(NR)]
        W2r = [abf(2 * 2048) for _ in range(NR)]
        actT = [abf(2 * 1024) for _ in range(2)]
        s1b = [alloc(512) for _ in range(2)]
        t1b = [alloc(512) for _ in range(2)]
        cbs = alloc(1024)
        sel_e = abf(128, 64)
        units = []
        for e in range(-1, N_EXP):
            for hf in range(2):
                units.append((e, hf))
        NU = len(units)

        def wsrc(e):
            return (dr["sw1"], dr["sw3"], dr["sw2"]) if e < 0 else (dr["ew1"][e], dr["ew3"][e], dr["ew2"][e])

        def load13(u):
            e, hf = units[u]
            s1_, s3_, _ = wsrc(e)
            W1, W3 = W1r[u % NR], W3r[u % NR]
            dma("pool", v3(W1, 16), s1_.rearrange("(c p) f -> p c f", p=128)[:, :, hf * 256:(hf + 1) * 256], [], [W1.r])
            dma("pool", v3(W3, 16), s3_.rearrange("(c p) f -> p c f", p=128)[:, :, hf * 256:(hf + 1) * 256], [], [W3.r])

        def load2(u):
            e, hf = units[u]
            s2_ = wsrc(e)[2]
            W2 = W2r[u % NR]
            dma("pool", v3(W2, 2), s2_[hf * 256:(hf + 1) * 256, :].rearrange("(c p) d -> p c d", p=128), [], [W2.r])

        gk = [0]
        yk = [0]
        ylast = [None]

        def ybank():
            b_ = PSB[4 + yk[0] % 4]
            ylast[0] = yk[0]
            yk[0] += 1
            return b_

        def ywr(b_):
            if ylast[0] % 2 == 0:
                return [b_.r, PSB[4 + (ylast[0] + 1) % 4].r]
            return [b_.r]

        def h_phase(u):
            e, hf = units[u]
            W1, W3, A_ = W1r[u % NR], W3r[u % NR], actT[u % 2]
            W1v, W3v, Av = v3(W1, 16), v3(W3, 16), v3(A_, 2)
            if e >= 0 and hf == 0:
                cp("dve", sel_e.ap, ID16[0:64, e:e + 1].to_broadcast([64, 128]), [cm16.r], [sel_e.r])
                for th in range(2):
                    pc_ = ybank()
                    mm(pc_.ap, sel_e.ap, combT.ap[:, th * 512:(th + 1) * 512], True, True, [sel_e.r, combT.r], ywr(pc_))
                    cp("act", cbs.ap[:, th * 512:(th + 1) * 512], pc_.ap, [pc_.r], [cbs.r])
            for fcl in range(2):
                for th in range(2):
                    p1, p3 = PSB[(gk[0] % 2) * 2], PSB[(gk[0] % 2) * 2 + 1]
                    sb1, tb1 = s1b[gk[0] % 2], t1b[gk[0] % 2]
                    gk[0] += 1
                    for kc in range(16):
                        mm(p1.ap, W1v[:, kc, fcl * 128:(fcl + 1) * 128], hmv[:, kc, th * 512:(th + 1) * 512], kc == 0, kc == 15,
                           [W1.r, hmT.r] + ([W3.r] if kc == 0 else []), [p1.r] + ([p3.r] if kc == 0 else []), sig=False)
                        if kc % 4 == 3:
                            yield
                    for kc in range(16):
                        mm(p3.ap, W3v[:, kc, fcl * 128:(fcl + 1) * 128], hmv[:, kc, th * 512:(th + 1) * 512], kc == 0, kc == 15,
                           [W3.r, hmT.r], [p3.r])
                        if kc % 4 == 3:
                            yield
                    act(sb1.ap, p1.ap, AF.Silu, [p1.r], [sb1.r])
                    if e < 0:
                        tt("dve", Av[:, fcl, th * 512:(th + 1) * 512], p3.ap, sb1.ap, ALU.mult, [p3.r, sb1.r], [A_.r])
                    else:
                        tt("dve", tb1.ap, p3.ap, sb1.ap, ALU.mult, [p3.r, sb1.r], [tb1.r])
                        tt("pool", Av[:, fcl, th * 512:(th + 1) * 512], tb1.ap, cbs.ap[:, th * 512:(th + 1) * 512], ALU.mult,
                           [tb1.r, cbs.r], [A_.r])

        def y_phase(u):
            W2, A_ = W2r[u % NR], actT[u % 2]
            W2v, Av = v3(W2, 2), v3(A_, 2)
            for dc in range(16):
                for th in range(2):
                    py = ybank()
                    for fcl in range(2):
                        mm(py.ap, W2v[:, fcl, dc * 128:(dc + 1) * 128], Av[:, fcl, th * 512:(th + 1) * 512], fcl == 0, fcl == 1,
                           [W2.r, A_.r], ywr(py) if fcl == 0 else [py.r], sig=(fcl == 1 and ylast[0] % 2 == 1))
                    ysl = yv[:, dc, th * 512:(th + 1) * 512]
                    stt("dve", ysl, py.ap, g2T.ap[:, dc:dc + 1], ysl, ALU.mult, ALU.add, [py.r, g2T.r, yacc.r], [yacc.r])
                    yield

        load13(0)
        load2(0)
        for u in range(NU + 1):
            if u + 1 < NU:
                load13(u + 1)
            gh = h_phase(u) if u < NU else iter(())
            gy = y_phase(u - 1) if u >= 1 else iter(())
            alive_h, alive_y = True, True
            while alive_h or alive_y:
                if alive_h:
                    try:
                        next(gh)
                    except StopIteration:
                        alive_h = False
                if alive_y:
                    try:
                        next(gy)
                    except StopIteration:
                        alive_y = False
            if u + 1 < NU:
                load2(u + 1)

        S.barrier()
        ot = [Buf(W1r[0].ap.bitcast(F32)), Buf(W3r[0].ap.bitcast(F32))]
        xr = [Buf(W2r[0].ap.bitcast(F32)), Buf(W1r[1].ap.bitcast(F32))]
        for i in range(8):
            O_, X_ = ot[i % 2], xr[i % 2]
            dma("sp", X_.ap, x1scr[i * 128:(i + 1) * 128, :], [x1s_r], [X_.r])
            for g4 in range(4):
                ps = PSB[(i * 4 + g4) % 8]
                for k4 in range(4):
                    dc = g4 * 4 + k4
                    tr(ps.ap[:, k4 * 128:(k4 + 1) * 128], yv[:, dc, i * 128:(i + 1) * 128], ID32, [yacc.r, cm32.r], [ps.r], k4 == 3)
                tt("dve", O_.ap[:, g4 * 512:(g4 + 1) * 512], ps.ap, X_.ap[:, g4 * 512:(g4 + 1) * 512], ALU.add, [ps.r, X_.r], [O_.r])
            dma("sp", out_d[i * 128:(i + 1) * 128, :], O_.ap, [O_.r], [])
        S.barrier()
        with nc.Block() as block:
            S.emit(block)
    return nc


_CACHE = {}


def kernel(**inputs):
    inp = {k: np.asarray(v, dtype=np.float32) for k, v in inputs.items()}
    sh = _prep_shared(inp)
    in_maps = []
    for c in range(8):
        m = dict(sh)
        m.update(_prep_core(inp, c))
        in_maps.append(m)
    shapes = {k: v.shape for k, v in in_maps[0].items()}
    nc = build_program(shapes)
    res = run_bass_kernel_spmd(nc, in_maps, core_ids=list(range(8)))
    out = np.zeros((2, 4096, 2048), np.float32)
    for c in range(8):
        out[c // 4, (c % 4) * 1024:(c % 4 + 1) * 1024] = res.results[c]["out"]
    if DEBUG:
        kernel.dbg = [{k: v for k, v in res.results[c].items() if k.startswith("dbg")} for c in range(8)]
    return out
```

```python
import numpy as np
from contextlib import ExitStack
import concourse.bass as bass
import concourse.mybir as mybir
from concourse.bass_utils import run_bass_kernel_spmd

F32 = mybir.dt.float32
BF16 = mybir.dt.bfloat16
AF = mybir.ActivationFunctionType
ALU = mybir.AluOpType
AX = mybir.AxisListType

D = 2048
KC = 16
NEG = -30000.0
EPS = 1e-6
DEBUG = False
N_EXP = 64
PHASE_MARKS = []


class Sched:
    CE = ("pe", "act", "dve", "pool")

    def __init__(self, nc, stack):
        self.nc = nc
        self.stack = stack
        self.ops = {k: [] for k in ("pe", "act", "dve", "pool", "sp")}
        self.cnt = {}
        self.seen = {k: {} for k in self.ops}
        self.sems = {}
        self.gen = 0
        self.cur = {}
        self.pending = {k: False for k in self.ops}
        self._new_compute_sems()
        self.qsems = {"sp": [], "pool": [], "act": []}
        self.qnext = {"sp": 0, "pool": 0, "act": 0}
        for q, n in (("sp", 16), ("pool", 12), ("act", 4)):
            for i in range(n):
                k = "d_%s%d" % (q, i)
                self.sems[k] = stack.enter_context(nc.semaphore(k))
                self.cnt[k] = 0
                self.qsems[q].append(k)

    def _new_compute_sems(self):
        for e in self.CE:
            k = "c_%s%d" % (e, self.gen)
            self.sems[k] = self.stack.enter_context(self.nc.semaphore(k))
            self.cnt[k] = 0
            self.cur[e] = k
        self.gen += 1

    @staticmethod
    def res():
        return {"w": None, "r": {}}

    def _deps(self, eng, reads, writes):
        deps = {}

        def add(k, c):
            if deps.get(k, 0) < c:
                deps[k] = c
        for r in reads:
            if r["w"] is not None:
                add(*r["w"])
        for w in writes:
            if w["w"] is not None:
                add(*w["w"])
            for k, c in w["r"].items():
                add(k, c)
        out = []
        for k, c in deps.items():
            if k == self.cur.get(eng) and c > self.cnt[k]:
                continue
            if self.seen[eng].get(k, 0) < c:
                self.seen[eng][k] = c
                out.append((k, c))
        return out

    def _mark(self, tok, reads, writes):
        k, c = tok
        for r in reads:
            if r["r"].get(k, 0) < c:
                r["r"][k] = c
        for w in writes:
            w["w"] = tok
            w["r"] = {}

    def op(self, eng, fn, reads=(), writes=(), signal=True):
        waits = self._deps(eng, reads, writes)
        k = self.cur[eng]
        if signal:
            self.cnt[k] += 1
            tok = (k, self.cnt[k])
            self.ops[eng].append((waits, fn, (k, 1)))
            self.pending[eng] = False
        else:
            tok = (k, self.cnt[k] + 1)
            self.ops[eng].append((waits, fn, None))
            self.pending[eng] = True
        self._mark(tok, reads, writes)
        return tok

    def dma(self, q, fn, reads=(), writes=()):
        sl = self.qsems[q]
        k = sl[self.qnext[q] % len(sl)]
        self.qnext[q] += 1
        waits = self._deps(q, reads, writes)
        prev = self.cnt[k]
        if prev > 0 and self.seen[q].get(k, 0) < prev:
            self.seen[q][k] = prev
            waits.append((k, prev))
        self.cnt[k] += 16
        tok = (k, self.cnt[k])
        self.ops[q].append((waits, fn, (k, 16)))
        self._mark(tok, reads, writes)
        return tok

    def barrier(self):
        assert not any(self.pending.values()), self.pending
        targets = [(k, c) for k, c in self.cnt.items() if c > 0]
        for e in self.ops:
            waits = []
            for k, c in targets:
                if self.seen[e].get(k, 0) < c:
                    self.seen[e][k] = c
                    waits.append((k, c))
            if waits:
                self.ops[e].append((waits, None, None))
        self._new_compute_sems()

    def emit(self, block):
        sems = self.sems

        def runner(name):
            def f(e):
                for waits, fn, inc in self.ops[name]:
                    for k, c in waits:
                        e.wait_ge(sems[k], c)
                    if fn is not None:
                        ins = fn(e)
                        if inc is not None:
                            ins.then_inc(sems[inc[0]], inc[1])
            return f
        block.tensor(runner("pe"))
        block.scalar(runner("act"))
        block.vector(runner("dve"))
        block.gpsimd(runner("pool"))
        block.sync(runner("sp"))


class Buf:
    def __init__(self, ap):
        self.ap = ap
        self.r = Sched.res()


def _swap_perm():
    idx = np.arange(128).reshape(2, 2, 32)
    return idx[:, ::-1, :].reshape(128)


def _rope_tables(pos_row, pos_col):
    inv = (10000.0 ** (-np.arange(32, dtype=np.float32) / 32.0)).astype(np.float32)
    ar = pos_row.astype(np.float32)[:, None] * inv[None, :]
    ac = pos_col.astype(np.float32)[:, None] * inv[None, :]
    cr, sr = np.cos(ar), np.sin(ar)
    cc, sc = np.cos(ac), np.sin(ac)
    cos = np.concatenate([cr, cr, cc, cc], axis=1).astype(np.float32)
    sin = np.concatenate([-sr, sr, -sc, sc], axis=1).astype(np.float32)
    return cos, sin


def _halo_chunks(q):
    hb = [8 * q - 2, 8 * q - 1] if q > 0 else [None, 3]
    ha = [8 * q + 8, 8 * q + 9] if q < 3 else [28, None]
    return hb + ha


def _na_bias(q, rpb):
    halo = _halo_chunks(q)
    out = np.full((8, 8, 128, 5, 128), NEG, np.float32)
    pk = np.arange(128)
    for i in range(8):
        tq = (8 * q + i) * 128 + np.arange(128)
        r, cq = tq // 64, tq % 64
        rs = np.clip(r - 4, 0, 56)
        cs = np.clip(cq - 8, 0, 48)
        seen = set()
        order = sorted(range(5), key=lambda j: (not (0 <= i - 2 + j <= 7), j))
        for j in order:
            sl = i - 2 + j
            if 0 <= sl <= 7:
                g = 8 * q + sl
            else:
                g = halo[{-2: 0, -1: 1, 8: 2, 9: 3}[sl]]
            if g is None or g in seen:
                continue
            seen.add(g)
            tk = g * 128 + pk
            rk, ck = tk // 64, tk % 64
            valid = ((rk[:, None] >= rs[None, :]) & (rk[:, None] < rs[None, :] + 8) &
                     (ck[:, None] >= cs[None, :]) & (ck[:, None] < cs[None, :] + 16))
            dr = np.clip(rk[:, None] - r[None, :] + 7, 0, 14)
            dc = np.clip(ck[:, None] - cq[None, :] + 15, 0, 30)
            vals = rpb[:, dr, dc]
            out[:, i, :, j, :] = np.where(valid[None], vals, NEG)
    return out.reshape(8, 8, 128, 640)


def _wlay(w, cw):
    n = w.shape[1]
    return np.ascontiguousarray(w.reshape(16, 128, n // cw, cw).transpose(2, 1, 0, 3))


def _prep_shared(inp):
    sh = {}
    w_in = inp["w_in"][0]
    o = np.cumsum([0, 1024, 1024, 1024, 512, 512, 1024, 1024, 16, 16])
    naq, nak, nav, gq, gk, gv, gg, df, db = [w_in[:, o[i]:o[i + 1]] for i in range(9)]
    perm = np.concatenate([h * 128 + _swap_perm() for h in range(4)])
    wdn = np.zeros((2048, 128), np.float32)
    wdn[:, 0:16] = df
    wdn[:, 32:48] = db
    wA = np.concatenate([naq, nak, gq, gq[:, perm], gk, gk[:, perm], gg, wdn, nav, gv], axis=1)
    sh["wA"] = _wlay(wA, 128)
    wB = np.concatenate([gk, gv], axis=1)
    sh["wB"] = _wlay(wB, 512)
    sh["wdn"] = np.ascontiguousarray(wdn[:, 0:64].reshape(16, 128, 64).transpose(1, 0, 2))
    sh["wmod"] = _wlay(inp["w_mod"][0], 512)
    sh["bmod2"] = np.ascontiguousarray(np.broadcast_to(inp["b_mod"][0][None], (2, 12288)))
    sh["n1g2"] = np.ascontiguousarray(np.broadcast_to(inp["norm1_g"][0][None], (2, 2048)))
    sh["n2g2"] = np.ascontiguousarray(np.broadcast_to(inp["norm2_g"][0][None], (2, 2048)))
    UP = np.zeros((64, 512), np.float32)
    UP[0:16] = inp["gla_gate_up_f"][0]
    UP[16] = inp["gla_gate_bias_f"][0]
    UP[32:48] = inp["gla_gate_up_b"][0]
    UP[48] = inp["gla_gate_bias_b"][0]
    sh["UP"] = UP
    sv = np.zeros((128, 8), np.float32)
    sv[:, 0] = inp["q_norm_g"][0]
    sv[:, 1] = inp["k_norm_g"][0]
    sv[:, 2] = inp["gla_norm_g"][0][0:128]
    sv[:, 3] = inp["gla_norm_g"][0][128:256]
    sv[16, 4] = 1.0
    sv[48, 4] = 1.0
    sv[:, 5] = -1.0 / 16.0
    sh["sv"] = sv
    sh["wout"] = inp["w_out"][0]
    sh["rw"] = np.ascontiguousarray(inp["router_w"][0].reshape(16, 128, 64).transpose(1, 0, 2))
    sh["rbias"] = np.ascontiguousarray(np.broadcast_to(inp["router_bias"][0][None], (128, 64)))
    sh["ew1"] = inp["expert_w1"][0]
    sh["ew3"] = inp["expert_w3"][0]
    sh["ew2"] = inp["expert_w2"][0]
    sh["sw1"] = inp["shared_w1"][0]
    sh["sw3"] = inp["shared_w3"][0]
    sh["sw2"] = inp["shared_w2"][0]
    i = np.arange(128)
    s_, t_ = i[:, None], i[None, :]
    c = np.float32(-1.0 / 16.0)
    cm = np.zeros((128, 8, 128), np.float32)
    cm[:, 0] = np.eye(128)
    cm[:, 1] = np.where(s_ > t_, c, 0)
    cm[:, 2] = np.where(s_ < t_, c, 0)
    cm[:, 3] = np.where(s_ <= t_, c, 0)
    cm[:, 4] = np.where(s_ >= t_, c, 0)
    cm[:, 5] = (s_ <= t_)
    cm[:, 6] = (s_ >= t_)
    cm[:, 7] = 1.0
    sh["cm"] = cm
    return sh


def _prep_core(inp, c):
    b, q = c // 4, c % 4
    x = inp["x"][b]
    ctx = inp["ctx"][b]
    m = {}
    m["xo"] = np.ascontiguousarray(x[q * 1024:(q + 1) * 1024])
    xh = np.zeros((512, 2048), np.float32)
    for s, g in enumerate(_halo_chunks(q)):
        if g is not None:
            xh[s * 128:(s + 1) * 128] = x[g * 128:(g + 1) * 128]
    m["xh"] = xh
    m["xc"] = np.ascontiguousarray(ctx)
    m["xg"] = np.concatenate([ctx[::-1], ctx, x[:q * 1024], x[(q + 1) * 1024:][::-1]], axis=0)
    cvec = np.stack([inp["c"][b], inp["c_ctx"]], axis=0)
    m["cT"] = np.ascontiguousarray(cvec.reshape(2, 16, 128).transpose(2, 1, 0))
    t_own = q * 1024 + np.arange(1024)
    cosO, sinO = _rope_tables(t_own // 64, t_own % 64)
    m["ropeO"] = np.ascontiguousarray(np.stack([cosO.reshape(8, 128, 128), sinO.reshape(8, 128, 128)], axis=1)
                                      .transpose(2, 0, 1, 3))
    m["ropeT"] = np.ascontiguousarray(np.stack([cosO.T, sinO.T], axis=1))
    t_lat = np.concatenate([np.arange(0, q * 1024), np.arange((q + 1) * 1024, 4096)[::-1]])
    cosG, sinG = _rope_tables(t_lat // 64, t_lat % 64)
    cosG = np.concatenate([np.ones((512, 128), np.float32), cosG], axis=0)
    sinG = np.concatenate([np.zeros((512, 128), np.float32), sinG], axis=0)
    m["ropeG"] = np.ascontiguousarray(np.stack([cosG.reshape(28, 128, 128), sinG.reshape(28, 128, 128)], axis=1)
                                      .transpose(2, 0, 1, 3))
    isf = np.concatenate([np.zeros(256), np.ones(256), np.ones(q * 1024), np.zeros((3 - q) * 1024)]).astype(np.float32)
    sel = np.zeros((64, 3584), np.float32)
    sel[0:32] = isf[None]
    sel[32:64] = 1.0 - isf[None]
    m["selG"] = sel
    fl = np.zeros((128, 8), np.float32)
    fl[:, q] = 1.0
    fl[:, 4:8] = 1.0 - fl[:, 0:4]
    m["flags"] = fl
    m["nabias"] = _na_bias(q, inp["na_rpb"][0])
    return m


def build_program(shapes):
    nc = bass.Bass("TRN2", target_bir_lowering=False)
    dr = {}
    for name, shp in shapes.items():
        dr[name] = nc.dram_tensor(name, list(shp), F32, kind="ExternalInput").ap()
    out_d = nc.dram_tensor("out", [1024, 2048], F32, kind="ExternalOutput").ap()
    modscr = nc.dram_tensor("modscr", [2, 12288], F32, kind="Internal").ap()
    x1scr = nc.dram_tensor("x1scr", [1024, 2048], F32, kind="Internal").ap()
    dbg = {}
    if DEBUG:
        dbg["mixT"] = nc.dram_tensor("dbg_mixT", [128, 16 * 1024], F32, kind="ExternalOutput").ap()
        dbg["x1"] = nc.dram_tensor("dbg_x1", [1024, 2048], F32, kind="ExternalOutput").ap()
        dbg["comb"] = nc.dram_tensor("dbg_comb", [1024, 64], F32, kind="ExternalOutput").ap()

    with ExitStack() as st:
        S = Sched(nc, st)
        AW = 47 * 1024
        arena = st.enter_context(nc.sbuf_tensor("arena", [128, AW], F32))
        top = [0]

        def alloc(words, dt=F32, parts=128):
            w = (words + 1) // 2 * 2
            assert top[0] + w <= AW, ("arena overflow", top[0], w)
            ap = arena[0:parts, top[0]:top[0] + w]
            top[0] += w
            if dt == BF16:
                ap = ap.bitcast(BF16)
            return Buf(ap)

        def abf(n, parts=128):
            return alloc((n + 1) // 2, BF16, parts)

        def release(mark):
            PHASE_MARKS.append(sum(1 for o in S.ops["dve"] if o[1] is not None))
            S.barrier()
            top[0] = mark

        PSB = []
        for i in range(8):
            PSB.append(Buf(st.enter_context(nc.psum_tensor("ps%d" % i, [128, 512], F32))[:]))

        def mm(out, lhsT, rhs, start, stop, rd, wr, sig=None):
            S.op("pe", lambda e: e.matmul(out, lhsT, rhs, start=start, stop=stop), reads=rd, writes=wr,
                 signal=bool(stop) if sig is None else sig)

        def tr(out, in_, ident, rd, wr, sig=True):
            S.op("pe", lambda e: e.transpose(out, in_, ident), reads=rd, writes=wr, signal=sig)

        def act(out, in_, func, rd, wr, **kw):
            S.op("act", lambda e: e.activation(out=out, in_=in_, func=func, **kw), reads=rd, writes=wr)

        def tt(eng, out, a, b, op, rd, wr):
            S.op(eng, lambda e: e.tensor_tensor(out, a, b, op), reads=rd, writes=wr)

        def ts(eng, out, a, s1, s2, op0, op1, rd, wr):
            if s2 is None:
                S.op(eng, lambda e: e.tensor_scalar(out, a, s1, None, op0), reads=rd, writes=wr)
            else:
                S.op(eng, lambda e: e.tensor_scalar(out, a, s1, s2, op0, op1), reads=rd, writes=wr)

        def stt(eng, out, a, sc, b, op0, op1, rd, wr):
            S.op(eng, lambda e: e.scalar_tensor_tensor(out, a, sc, b, op0, op1), reads=rd, writes=wr)

        def cp(eng, out, in_, rd, wr):
            if eng == "act":
                act(out, in_, AF.Copy, rd, wr)
            else:
                S.op(eng, lambda e: e.tensor_copy(out, in_), reads=rd, writes=wr)

        def dma(q, out, in_, rd, wr):
            return S.dma(q, lambda e: e.dma_start(out=out, in_=in_), reads=rd, writes=wr)

        def memset(eng, out, val, wr):
            S.op(eng, lambda e: e.memset(out, val), writes=wr)

        cm32 = alloc(8 * 128)
        dma("sp", cm32.ap.rearrange("p (a b) -> p a b", a=8), dr["cm"], [], [cm32.r])
        cmv = cm32.ap.rearrange("p (a b) -> p a b", a=8)
        ID32, TU, TLS, TLI, TUI = (cmv[:, k, :] for k in range(5))
        cm16 = abf(8 * 128)
        dma("pool", cm16.ap.rearrange("p (a b) -> p a b", a=8), dr["cm"], [], [cm16.r])
        cmb = cm16.ap.rearrange("p (a b) -> p a b", a=8)
        ID16, MF16, MB16, ONES16 = cmb[:, 0, :], cmb[:, 5, :], cmb[:, 6, :], cmb[:, 7, :]
        MF32, MB32 = cmv[:, 5, :], cmv[:, 6, :]
        sv = alloc(8)
        dma("sp", sv.ap, dr["sv"], [], [sv.r])
        QNG, KNG, GNG0, GNG1, DBIAS, NEG16 = (sv.ap[:, k:k + 1] for k in range(6))
        flags = alloc(8)
        dma("sp", flags.ap, dr["flags"], [], [flags.r])
        UP = alloc(512, F32, 64)
        dma("sp", UP.ap, dr["UP"], [], [UP.r])
        CR = [cm32.r, cm16.r, sv.r]
        stat = [alloc(4) for _ in range(4)]
        stat_i = [0]

        cTs = abf(32)
        m0 = top[0]
        cT = alloc(32)
        dma("sp", cT.ap.rearrange("p (a b) -> p a b", a=16), dr["cT"], [], [cT.r])
        act(cTs.ap, cT.ap, AF.Silu, [cT.r], [cTs.r])
        cTv = cTs.ap.rearrange("p (a b) -> p a b", a=16)
        modrows = alloc(4096, F32, 2)
        wm = [abf(16 * 512) for _ in range(2)]
        bm = [alloc(512, F32, 2) for _ in range(2)]
        for cb in range(8):
            W = wm[cb % 2]
            B = bm[cb % 2]
            ps = PSB[cb % 2]
            dma("pool", W.ap, dr["wmod"][cb].rearrange("p a b -> p (a b)"), [], [W.r])
            dma("sp", B.ap, dr["bmod2"][:, cb * 512:(cb + 1) * 512], [], [B.r])
            Wv = W.ap.rearrange("p (a b) -> p a b", a=16)
            for kc in range(16):
                mm(ps.ap[0:2, :], cTv[:, kc, :], Wv[:, kc, :], kc == 0, kc == 15, [cTs.r, W.r], [ps.r])
            tt("dve", modrows.ap[:, cb * 512:(cb + 1) * 512], ps.ap[0:2, :], B.ap, ALU.add, [ps.r, B.r], [modrows.r])
        ng = alloc(2048, F32, 2)
        dma("sp", ng.ap, dr["n1g2"], [], [ng.r])
        sl = modrows.ap[:, 2048:4096]
        stt("dve", sl, sl, 1.0, ng.ap, ALU.add, ALU.mult, [modrows.r, ng.r], [modrows.r])
        rms = Sched.res()
        dma("sp", modscr[:, 0:4096], modrows.ap, [modrows.r], [rms])
        release(m0)

        def load_bcast(buf, row, sec):
            dma("sp", buf.ap, modscr[row:row + 1, sec * 2048:(sec + 1) * 2048].to_broadcast([128, 2048]), [rms], [buf.r])

        def rstd_from_ssq(stb, n):
            ts("dve", stb.ap[:, 1:2], stb.ap[:, 0:1], 1.0 / n, EPS, ALU.mult, ALU.add, [stb.r], [stb.r])
            act(stb.ap[:, 1:2], stb.ap[:, 1:2], AF.Ln, [stb.r], [stb.r])
            act(stb.ap[:, 1:2], stb.ap[:, 1:2], AF.Exp, [stb.r], [stb.r], scale=-0.5)

        class HT:
            def __init__(self):
                self.xt = [alloc(2048) for _ in range(2)]
                self.junk = abf(2048)
                self.xn = [abf(2048) for _ in range(2)]
                self.i = 0

            def elem(self, src, gm, shf):
                idx = self.i
                xt = self.xt[idx % 2]
                xn = self.xn[idx % 2]
                self.i += 1
                stb = stat[stat_i[0] % 4]
                stat_i[0] += 1
                dma("sp", xt.ap, src, [], [xt.r])
                memset("dve", stb.ap, 0.0, [stb.r])
                act(self.junk.ap, xt.ap, AF.Square, [xt.r], [self.junk.r, stb.r], accum_out=stb.ap[:, 0:1])
                rstd_from_ssq(stb, 2048)
                stt("dve", xt.ap, xt.ap, stb.ap[:, 1:2], gm.ap, ALU.mult, ALU.mult, [xt.r, stb.r, gm.r], [xt.r])
                tt("dve", xn.ap, xt.ap, shf.ap, ALU.add, [xt.r, shf.r], [xn.r])
                return idx

            def trs(self, idx, dst_ap, dst_r, psA, psB):
                xn = self.xn[idx % 2]
                for half, ps in ((0, psA), (1, psB)):
                    pv = ps.ap.bitcast(BF16).rearrange("p (a b) -> p a b", a=8)
                    for k8 in range(8):
                        kc = half * 8 + k8
                        tr(pv[:, k8, :], xn.ap[:, kc * 128:(kc + 1) * 128], ID16, [xn.r, cm16.r], [ps.r], k8 == 7)
                    cp("act" if half == 0 else "dve", dst_ap[:, half * 8:(half + 1) * 8, :], pv, [ps.r], [dst_r])

            def make(self, src, gm, shf, dst_ap, dst_r, psA, psB):
                self.trs(self.elem(src, gm, shf), dst_ap, dst_r, psA, psB)

        def v3(buf, a):
            return buf.ap.rearrange("p (a b) -> p a b", a=a)

        def rope_tok(kps_ap, kps_r, cos_ap, sin_ap, tab_r, out, t2, nh):
            cosb = cos_ap.rearrange("p (o c) -> p o c", o=1).to_broadcast([128, nh, 128])
            tt("dve", out.ap.rearrange("p (h c) -> p h c", h=nh), kps_ap.rearrange("p (h c) -> p h c", h=nh), cosb, ALU.mult,
               [kps_r, tab_r], [out.r])
            s4 = sin_ap.rearrange("p (a b c) -> p a b c", a=2, b=2)
            for bb in range(2):
                for a in range(2):
                    sb = s4[:, a, bb, :].rearrange("p (o c) -> p o c", o=1).to_broadcast([128, nh, 32])
                    kin = kps_ap.rearrange("p (h a b c) -> p h a b c", h=nh, a=2, b=2)[:, :, a, 1 - bb, :]
                    tout = t2.ap.rearrange("p (h a b c) -> p h a b c", h=nh, a=2, b=2)[:, :, a, bb, :]
                    tt("dve", tout, kin, sb, ALU.mult, [kps_r, tab_r], [t2.r])
            tt("pool", out.ap, out.ap, t2.ap, ALU.add, [out.r, t2.r], [out.r])

        Sst = alloc(1024)
        Scb = alloc(1024)
        Sfs = alloc(1024)
        memset("dve", Sst.ap, 0.0, [Sst.r])
        memset("dve", Sfs.ap, 0.0, [Sfs.r])
        mA = top[0]
        gm1L, sh1L, gm1C, sh1C = (alloc(2048) for _ in range(4))
        load_bcast(gm1L, 0, 1)
        load_bcast(sh1L, 0, 0)
        load_bcast(gm1C, 1, 1)
        load_bcast(sh1C, 1, 0)
        ht = HT()
        wkv = abf(16 * 1536)
        wkvv = v3(wkv, 16)
        for blk in range(3):
            dma("pool", wkvv[:, :, blk * 512:(blk + 1) * 512], dr["wB"][blk], [], [wkv.r])
        wdn = abf(16 * 64)
        wdnv = v3(wdn, 16)
        dma("pool", wdnv, dr["wdn"], [], [wdn.r])
        hTc = [abf(16 * 128) for _ in range(2)]
        vt = [abf(1024) for _ in range(2)]
        rp = [alloc(256) for _ in range(2)]
        krb = [alloc(512) for _ in range(2)]
        t2 = alloc(512)
        Dtb = [alloc(128, F32, 64) for _ in range(2)]
        selG = alloc(3584, F32, 64)
        dma("sp", selG.ap, dr["selG"], [], [selG.r])
        e1 = alloc(512)
        sp_ = alloc(512)
        Eb = alloc(512)
        kd = abf(512)
        dec = alloc(4)

        def checkpoint(col):
            m_, nm_ = flags.ap[:, col:col + 1], flags.ap[:, 4 + col:5 + col]
            stt("dve", Sfs.ap, Sst.ap, m_, Sfs.ap, ALU.mult, ALU.add, [Sst.r, Sfs.r, flags.r], [Sfs.r])
            ts("dve", Sst.ap, Sst.ap, nm_, None, ALU.mult, None, [Sst.r, flags.r], [Sst.r])
            stt("dve", Sst.ap, Scb.ap, m_, Sst.ap, ALU.mult, ALU.add, [Scb.r, Sst.r, flags.r], [Sst.r])

        def s1_elem(j):
            xt = ht.xt[j % 2]
            xn = ht.xn[j % 2]
            stb = stat[stat_i[0] % 4]
            stat_i[0] += 1
            gm, shf = (gm1C, sh1C) if j < 4 else (gm1L, sh1L)
            dma("sp", xt.ap, dr["xg"][j * 128:(j + 1) * 128, :], [], [xt.r])
            dma("sp", v3(rp[j % 2], 2), dr["ropeG"][:, j], [], [rp[j % 2].r])
            memset("dve", stb.ap, 0.0, [stb.r])
            act(ht.junk.ap, xt.ap, AF.Square, [xt.r], [ht.junk.r, stb.r], accum_out=stb.ap[:, 0:1])
            rstd_from_ssq(stb, 2048)
            stt("dve", xt.ap, xt.ap, stb.ap[:, 1:2], gm.ap, ALU.mult, ALU.mult, [xt.r, stb.r, gm.r], [xt.r])
            tt("dve", xn.ap, xt.ap, shf.ap, ALU.add, [xt.r, shf.r], [xn.r])

        def s1_tr(j):
            xn = ht.xn[j % 2]
            H = hTc[j % 2]
            Hv = v3(H, 16)
            ps = PSB[0]
            pv = ps.ap.bitcast(BF16).rearrange("p (a b) -> p a b", a=8)
            for half in range(2):
                for k8 in range(8):
                    kc = half * 8 + k8
                    tr(pv[:, k8, :], xn.ap[:, kc * 128:(kc + 1) * 128], ID16, [xn.r, cm16.r], [ps.r], k8 == 7)
                cp("act" if half == 0 else "dve", Hv[:, half * 8:(half + 1) * 8, :], pv, [ps.r], [H.r])

        def s3_z(j):
            Dt = Dtb[j % 2]
            mm(PSB[6].ap, Dt.ap, UP.ap, True, True, [Dt.r, UP.r], [PSB[6].r])
            act(e1.ap, PSB[6].ap, AF.Exp, [PSB[6].r], [e1.r], scale=-1.0)
            act(sp_.ap, e1.ap, AF.Ln, [e1.r], [sp_.r], bias=1.0, scale=1.0)

        def s3_rest(j):
            mm(PSB[6].ap, TU, sp_.ap, True, True, [sp_.r, cm32.r], [PSB[6].r])
            for h in range(4):
                mm(PSB[5].ap[:, 128 + h:129 + h], sp_.ap[:, h * 128:(h + 1) * 128], NEG16, True, True, [sp_.r, sv.r], [PSB[5].r], sig=(h == 3))
            act(Eb.ap, PSB[6].ap, AF.Exp, [PSB[6].r], [Eb.r])
            act(dec.ap, PSB[5].ap[:, 128:132], AF.Exp, [PSB[5].r], [dec.r])
            tt("dve", kd.ap, krb[j % 2].ap, Eb.ap, ALU.mult, [krb[j % 2].r, Eb.r], [kd.r])

        def s3_state(j):
            V_ = vt[j % 2]
            for h in range(4):
                ps = PSB[1] if h < 2 else PSB[7]
                mm(ps.ap[:, (h % 2) * 256:(h % 2 + 1) * 256], kd.ap[:, h * 128:(h + 1) * 128], V_.ap[:, h * 256:(h + 1) * 256],
                   True, True, [kd.r, V_.r], [ps.r], sig=(h == 3))
            for h in range(4):
                ps = PSB[1] if h < 2 else PSB[7]
                stt("dve", Sst.ap[:, h * 256:(h + 1) * 256], Sst.ap[:, h * 256:(h + 1) * 256], dec.ap[:, h:h + 1],
                    ps.ap[:, (h % 2) * 256:(h % 2 + 1) * 256], ALU.mult, ALU.add, [Sst.r, dec.r, ps.r], [Sst.r])
            if j == 1:
                cp("dve", Scb.ap, Sst.ap, [Sst.r], [Scb.r])
                memset("dve", Sst.ap, 0.0, [Sst.r])
            if j in (3, 11, 19, 27):
                checkpoint((j - 3) // 8)

        Wbg = abf(16 * 256)
        bbg = [alloc(256, F32, 2) for _ in range(2)]
        nbg = [alloc(256, F32, 2) for _ in range(2)]
        rbg = [alloc(256, F32, 2) for _ in range(2)]
        bg_i = [0]

        def adaln_bg():
            hb = bg_i[0]
            if hb >= 32:
                return
            bg_i[0] += 1
            c0 = 4096 + hb * 256
            blk, half = c0 // 512, (c0 % 512) // 256
            B_, N_, R_ = bbg[hb % 2], nbg[hb % 2], rbg[hb % 2]
            Wv_ = v3(Wbg, 16)
            dma("pool", Wv_, dr["wmod"][blk][:, :, half * 256:(half + 1) * 256], [], [Wbg.r])
            dma("sp", B_.ap, dr["bmod2"][:, c0:c0 + 256], [], [B_.r])
            reg = PSB[5].ap[0:2, 256:512]
            for kc in range(16):
                mm(reg, cTv[:, kc, :], Wv_[:, kc, :], kc == 0, kc == 15, [cTs.r, Wbg.r], [PSB[5].r])
            tt("dve", R_.ap, reg, B_.ap, ALU.add, [PSB[5].r, B_.r], [R_.r])
            if 8192 <= c0 < 10240:
                dma("sp", N_.ap, dr["n2g2"][:, c0 - 8192:c0 - 8192 + 256], [], [N_.r])
                stt("dve", R_.ap, R_.ap, 1.0, N_.ap, ALU.add, ALU.mult, [R_.r, N_.r], [R_.r])
            dma("sp", modscr[:, c0:c0 + 256], R_.ap, [R_.r], [rms])

        s1_elem(0)
        s1_tr(0)
        for j in range(29):
            adaln_bg()
            if j < 3:
                adaln_bg()
            if j < 28:
                H = hTc[j % 2]
                Hv = v3(H, 16)
                V_, R_, KR, Dt = vt[j % 2], rp[j % 2], krb[j % 2], Dtb[j % 2]
                for kc in range(16):
                    mm(PSB[2].ap, Hv[:, kc, :], wkvv[:, kc, 0:512], kc == 0, kc == 15, [H.r, wkv.r], [PSB[2].r])
                rope_tok(PSB[2].ap, PSB[2].r, R_.ap[:, 0:128], R_.ap[:, 128:256], R_.r, KR, t2, 4)
            if j + 1 < 28:
                s1_elem(j + 1)
            if j >= 1:
                s3_z(j - 1)
            if j < 28:
                for kc in range(16):
                    mm(PSB[3].ap, Hv[:, kc, :], wkvv[:, kc, 512:1024], kc == 0, kc == 15, [H.r, wkv.r], [PSB[3].r])
                cp("act", V_.ap[:, 0:512], PSB[3].ap, [PSB[3].r], [V_.r])
            if j >= 1:
                s3_rest(j - 1)
            if j < 28:
                for kc in range(16):
                    mm(PSB[4].ap, Hv[:, kc, :], wkvv[:, kc, 1024:1536], kc == 0, kc == 15, [H.r, wkv.r], [PSB[4].r])
                cp("act", V_.ap[:, 512:1024], PSB[4].ap, [PSB[4].r], [V_.r])
            if j >= 1:
                s3_state(j - 1)
            if j < 28:
                for kc in range(16):
                    mm(PSB[5].ap[0:64, 0:128], wdnv[:, kc, :], Hv[:, kc, :], kc == 0, kc == 15, [H.r, wdn.r], [PSB[5].r])
                stt("dve", Dt.ap, PSB[5].ap[0:64, 0:128], DBIAS[0:64, :], selG.ap[:, j * 128:(j + 1) * 128], ALU.add, ALU.mult,
                    [PSB[5].r, sv.r, selG.r], [Dt.r])
            if j + 1 < 28:
                s1_tr(j + 1)
        release(mA)

        mixT = abf(16 * 1024)
        mixv = v3(mixT, 16)
        hTown = abf(16 * 1024)
        hTo = v3(hTown, 16)
        hTr = [Sched.res() for _ in range(8)]
        hT6 = abf(16 * 768)
        hT6v = v3(hT6, 16)
        hT6r = [Sched.res() for _ in range(6)]
        mB = top[0]
        gm1L, sh1L, gm1C, sh1C = (alloc(2048) for _ in range(4))
        load_bcast(gm1L, 0, 1)
        load_bcast(sh1L, 0, 0)
        load_bcast(gm1C, 1, 1)
        load_bcast(sh1C, 1, 0)
        ht = HT()

        def b_args(j):
            if j < 8:
                return (dr["xo"][j * 128:(j + 1) * 128, :], gm1L, sh1L, hTo[:, :, j * 128:(j + 1) * 128], hTr[j])
            jj = j - 8
            src = dr["xh"][jj * 128:(jj + 1) * 128, :] if jj < 4 else dr["xc"][(jj - 4) * 128:(jj - 3) * 128, :]
            return (src, gm1L if jj < 4 else gm1C, sh1L if jj < 4 else sh1C, hT6v[:, :, jj * 128:(jj + 1) * 128], hT6r[jj])

        ids = {}
        a0 = b_args(0)
        ids[0] = ht.elem(a0[0], a0[1], a0[2])
        for j in range(14):
            if j + 1 < 14:
                a1 = b_args(j + 1)
                ids[j + 1] = ht.elem(a1[0], a1[1], a1[2])
            aj = b_args(j)
            ht.trs(ids[j], aj[3], aj[4], PSB[(2 * j) % 8], PSB[(2 * j + 1) % 8])
        release(mB)

        def load_w(buf, idx):
            dma("pool", buf.ap, dr["wA"][idx].rearrange("p a b -> p (a b)"), [], [buf.r])
            return v3(buf, 16)

        qTh = [abf(1024) for _ in range(2)]
        kTh = [abf(1792) for _ in range(2)]
        vh = [abf(14 * 128) for _ in range(2)]
        wq_ = [abf(16 * 128) for _ in range(2)]
        wk_ = [abf(16 * 128) for _ in range(2)]
        wv_ = [abf(16 * 128) for _ in range(2)]
        sq16 = [abf(512) for _ in range(2)]
        rs32 = [alloc(512) for _ in range(2)]
        bt = [alloc(640) for _ in range(2)]
        sA = [alloc(640) for _ in range(2)]
        PT = [abf(896) for _ in range(2)]
        rden = [alloc(128) for _ in range(2)]
        it = [0]
        scale = 128.0 ** -0.5

        def qk_norm_unit(Wv, W, hsrc, hres, n, dst_ap, dst_r, gvec, pa, pb):
            k = it[0]
            it[0] += 1
            sq, rs = sq16[k % 2], rs32[k % 2]
            for kc in range(16):
                mm(pa.ap[:, 0:n], Wv[:, kc, :], hsrc[:, kc, :], kc == 0, kc == 15, [W.r] + hres, [pa.r])
            act(sq.ap[:, 0:n], pa.ap[:, 0:n], AF.Square, [pa.r], [sq.r])
            mm(pb.ap[:, 0:n], ONES16, sq.ap[:, 0:n], True, True, [sq.r, cm16.r], [pb.r])
            act(rs.ap[:, 0:n], pb.ap[:, 0:n], AF.Ln, [pb.r], [rs.r], scale=1.0 / 128, bias=EPS)
            act(rs.ap[:, 0:n], rs.ap[:, 0:n], AF.Exp, [rs.r], [rs.r], scale=-0.5)
            stt("dve", dst_ap, pa.ap[:, 0:n], gvec, rs.ap[:, 0:n], ALU.mult, ALU.mult, [pa.r, rs.r, sv.r], [dst_r])

        def na_proj(h):
            hp = h % 2
            Q_, K_, V_ = qTh[hp], kTh[hp], vh[hp]
            Wq, Wk, Wvv_ = wq_[hp], wk_[hp], wv_[hp]
            Wqv, Wkv, Wvv = load_w(Wq, h), load_w(Wk, 8 + h), load_w(Wvv_, 41 + h)
            for g in range(2):
                qk_norm_unit(Wqv, Wq, hTo[:, :, g * 512:(g + 1) * 512], hTr[g * 4:(g + 1) * 4], 512,
                             Q_.ap[:, g * 512:(g + 1) * 512], Q_.r, QNG, PSB[0], PSB[1])
                yield
            for g in range(2):
                qk_norm_unit(Wkv, Wk, hTo[:, :, g * 512:(g + 1) * 512], hTr[g * 4:(g + 1) * 4], 512,
                             K_.ap[:, g * 512:(g + 1) * 512], K_.r, KNG, PSB[0], PSB[1])
                yield
            qk_norm_unit(Wkv, Wk, hT6v[:, :, 0:512], hT6r[0:4], 512, K_.ap[:, 1024:1536], K_.r, KNG, PSB[0], PSB[1])
            yield
            qk_norm_unit(Wkv, Wk, hT6v[:, :, 512:768], hT6r[4:6], 256, K_.ap[:, 1536:1792], K_.r, KNG, PSB[0], PSB[1])
            yield
            for c4 in range(4):
                ps = PSB[c4 % 2]
                chs = list(range(c4 * 4, min(c4 * 4 + 4, 14)))
                for jj, ch in enumerate(chs):
                    if ch < 8:
                        src, sres = hTo[:, :, ch * 128:(ch + 1) * 128], [hTr[ch]]
                    else:
                        src, sres = hT6v[:, :, (ch - 8) * 128:(ch - 7) * 128], [hT6r[ch - 8]]
                    for kc in range(16):
                        mm(ps.ap[:, jj * 128:(jj + 1) * 128], src[:, kc, :], Wvv[:, kc, :], kc == 0, kc == 15, [Wvv_.r] + sres, [ps.r])
                cp("act", V_.ap[:, chs[0] * 128:(chs[-1] + 1) * 128], ps.ap[:, 0:len(chs) * 128], [ps.r], [V_.r])
                yield

        def na_chunks(i):
            chunks = []
            for j in range(5):
                sl = i - 2 + j
                chunks.append(sl if 0 <= sl <= 7 else {-2: 8, -1: 9, 8: 10, 9: 11}[sl])
            return chunks + [12, 13]

        def na_att(h):
            hp = h % 2
            Q_, K_, V_ = qTh[hp], kTh[hp], vh[hp]
            Vv = v3(V_, 14)

            def stage1(i):
                par = i % 2
                bA, bB = PSB[2 + 2 * par], PSB[3 + 2 * par]
                B_, SA_, PT_ = bt[par], sA[par], PT[par]
                dma("sp", B_.ap, dr["nabias"][h, i], [], [B_.r])
                qs = Q_.ap[:, i * 128:(i + 1) * 128]
                for j, ch in enumerate(na_chunks(i)):
                    dst = bA.ap[:, j * 128:(j + 1) * 128] if j < 4 else bB.ap[:, (j - 4) * 128:(j - 3) * 128]
                    mm(dst, K_.ap[:, ch * 128:(ch + 1) * 128], qs, True, True, [K_.r, Q_.r], [bA.r if j < 4 else bB.r],
                       sig=(j in (3, 6)))
                stt("dve", SA_.ap[:, 0:512], bA.ap, scale, B_.ap[:, 0:512], ALU.mult, ALU.add, [bA.r, B_.r], [SA_.r])
                stt("dve", SA_.ap[:, 512:640], bB.ap[:, 0:128], scale, B_.ap[:, 512:640], ALU.mult, ALU.add, [bB.r, B_.r], [SA_.r])
                act(PT_.ap[:, 0:640], SA_.ap, AF.Exp, [SA_.r], [PT_.r])
                act(PT_.ap[:, 640:896], bB.ap[:, 128:384], AF.Exp, [bB.r], [PT_.r], scale=scale)

            def stage2(i):
                par = i % 2
                bC, bD = PSB[6], PSB[7]
                PT_, RD_ = PT[par], rden[par]
                chunks = na_chunks(i)
                for j, ch in enumerate(chunks):
                    mm(bC.ap[:, 0:128], Vv[:, ch, :], PT_.ap[:, j * 128:(j + 1) * 128], j == 0, j == 6, [V_.r, PT_.r], [bC.r], sig=False)
                for j, ch in enumerate(chunks):
                    mm(bD.ap[:, 0:128], ONES16, PT_.ap[:, j * 128:(j + 1) * 128], j == 0, j == 6, [cm16.r, PT_.r], [bD.r])
                S.op("dve", lambda e, o=RD_.ap, a=bD.ap[:, 0:128]: e.reciprocal(o, a), reads=[bD.r], writes=[RD_.r])
                tt("dve", mixv[:, h, i * 128:(i + 1) * 128], bC.ap[:, 0:128], RD_.ap, ALU.mult, [bC.r, RD_.r], [mixT.r])

            stage1(0)
            yield
            for i in range(8):
                if i + 1 < 8:
                    stage1(i + 1)
                    yield
                stage2(i)
                yield

        def interleave(g1, g2):
            a1, a2 = True, True
            while a1 or a2:
                if a1:
                    try:
                        next(g1)
                    except StopIteration:
                        a1 = False
                if a2:
                    try:
                        next(g2)
                    except StopIteration:
                        a2 = False

        interleave(na_proj(0), iter(()))
        for h in range(8):
            interleave(na_att(h), na_proj(h + 1) if h + 1 < 8 else iter(()))
        release(mB - (16 * 768) // 2)

        wdn = abf(16 * 64)
        wdnv = v3(wdn, 16)
        dma("pool", wdnv, dr["wdn"], [], [wdn.r])
        DtO = alloc(1024, F32, 64)
        for g in range(2):
            pa = PSB[g]
            hs = hTo[:, :, g * 512:(g + 1) * 512]
            for kc in range(16):
                mm(pa.ap[0:64, :], wdnv[:, kc, :], hs[:, kc, :], kc == 0, kc == 15, [wdn.r] + hTr[g * 4:(g + 1) * 4], [pa.r])
            ts("dve", DtO.ap[:, g * 512:(g + 1) * 512], pa.ap[0:64, :], DBIAS[0:64, :], None, ALU.add, None, [pa.r, sv.r], [DtO.r])
        ropeT = alloc(2048)
        dma("sp", v3(ropeT, 2), dr["ropeT"], [], [ropeT.r])
        rTv = v3(ropeT, 2)
        ropeO = alloc(8 * 256)
        dma("sp", ropeO.ap.rearrange("p (a b c) -> p a b c", a=8, b=2), dr["ropeO"], [], [ropeO.r])
        rOv = v3(ropeO, 8)
        WG = [abf(16 * 128) for _ in range(8)]
        qr = abf(1024)
        krf = abf(1024)
        gate = abf(2 * 1024)
        gav = v3(gate, 2)
        vown = abf(8 * 256)
        vov = v3(vown, 8)
        kdF, kdB = abf(8 * 128), abf(8 * 128)
        QF, KF, QB, KB = (abf(8 * 128) for _ in range(4))
        SfT, SbT = abf(8 * 256), abf(8 * 256)
        decO = alloc(16)
        tmpa = [alloc(512)] * 2
        tmpb = [alloc(512)] * 2
        kr1b = [alloc(128) for _ in range(2)]
        t21 = alloc(128)
        e11, sp1, Eb1, epos, eneg = ([alloc(128) for _ in range(2)] for _ in range(5))
        AFb = [abf(128) for _ in range(2)]
        ABb = [abf(128) for _ in range(2)]
        oT = [alloc(256) for _ in range(2)]
        osq = [abf(256) for _ in range(2)]
        rsg = [alloc(128) for _ in range(2)]
        qscale = 128.0 ** -0.5
        kk = 0
        for h in range(4):
            Wq, Wqs, Wk, Wks, Wg0, Wg1, Wv0, Wv1 = WG
            Wqv, Wqsv, Wkv, Wksv = load_w(Wq, 16 + h), load_w(Wqs, 20 + h), load_w(Wk, 24 + h), load_w(Wks, 28 + h)
            Wg0v, Wg1v = load_w(Wg0, 32 + 2 * h), load_w(Wg1, 33 + 2 * h)
            Wv0v, Wv1v = load_w(Wv0, 49 + 2 * h), load_w(Wv1, 50 + 2 * h)
            for (Wa, Wav, Wb, Wbv, dst) in ((Wq, Wqv, Wqs, Wqsv, qr), (Wk, Wkv, Wks, Wksv, krf)):
                for g in range(2):
                    pa, pb = PSB[(2 * kk) % 8], PSB[(2 * kk + 1) % 8]
                    ta, tb = tmpa[kk % 2], tmpb[kk % 2]
                    kk += 1
                    hs = hTo[:, :, g * 512:(g + 1) * 512]
                    for kc in range(16):
                        mm(pa.ap, Wav[:, kc, :], hs[:, kc, :], kc == 0, kc == 15, [Wa.r] + hTr[g * 4:(g + 1) * 4], [pa.r])
                    for kc in range(16):
                        mm(pb.ap, Wbv[:, kc, :], hs[:, kc, :], kc == 0, kc == 15, [Wb.r] + hTr[g * 4:(g + 1) * 4], [pb.r])
                    tt("dve", ta.ap, pa.ap, rTv[:, 0, g * 512:(g + 1) * 512], ALU.mult, [pa.r, ropeT.r], [ta.r])
                    tt("dve", tb.ap, pb.ap, rTv[:, 1, g * 512:(g + 1) * 512], ALU.mult, [pb.r, ropeT.r], [tb.r])
                    tt("pool", dst.ap[:, g * 512:(g + 1) * 512], ta.ap, tb.ap, ALU.add, [ta.r, tb.r], [dst.r])
            for vc, (Wg, Wgv) in enumerate(((Wg0, Wg0v), (Wg1, Wg1v))):
                for g in range(2):
                    pa = PSB[kk % 8]
                    kk += 1
                    hs = hTo[:, :, g * 512:(g + 1) * 512]
                    for kc in range(16):
                        mm(pa.ap, Wgv[:, kc, :], hs[:, kc, :], kc == 0, kc == 15, [Wg.r] + hTr[g * 4:(g + 1) * 4], [pa.r])
                    act(gav[:, vc, g * 512:(g + 1) * 512], pa.ap, AF.Silu, [pa.r], [gate.r])
            def d_pstage(i):
                tok = slice(i * 128, (i + 1) * 128)
                Hs = hTo[:, :, tok]
                pk, pv = PSB[0], PSB[1]
                for kc in range(16):
                    mm(pk.ap[:, 0:128], Hs[:, kc, :], Wkv[:, kc, :], kc == 0, kc == 15, [hTr[i], Wk.r], [pk.r])
                for vc, (Wv_, Wvv_) in enumerate(((Wv0, Wv0v), (Wv1, Wv1v))):
                    for kc in range(16):
                        mm(pv.ap[:, vc * 128:(vc + 1) * 128], Hs[:, kc, :], Wvv_[:, kc, :], kc == 0, kc == 15, [hTr[i], Wv_.r], [pv.r])
                cp("act", vov[:, i, :], pv.ap[:, 0:256], [pv.r], [vown.r])
                rope_tok(pk.ap[:, 0:128], pk.r, rOv[:, i, 0:128], rOv[:, i, 128:256], ropeO.r, kr1b[i % 2], t21, 1)

            def d_chains(i):
                tok = slice(i * 128, (i + 1) * 128)
                kr1 = kr1b[i % 2]
                dirs = ((0, TU, TLI, kdF, QF, KF), (32, TLS, TUI, kdB, QB, KB))
                pz, prs, pc = (PSB[2], PSB[3]), (PSB[4], PSB[5]), (PSB[6], PSB[7])
                for d_, (lo, TR, TC, kdb, Qb, Kb) in enumerate(dirs):
                    mm(pz[d_].ap[:, 0:128], DtO.ap[lo:lo + 32, tok], UP.ap[lo:lo + 32, h * 128:(h + 1) * 128], True, True,
                       [DtO.r, UP.r], [pz[d_].r], sig=(d_ == 1))
                for d_ in range(2):
                    act(e11[d_].ap, pz[d_].ap[:, 0:128], AF.Exp, [pz[d_].r], [e11[d_].r], scale=-1.0)
                for d_ in range(2):
                    act(sp1[d_].ap, e11[d_].ap, AF.Ln, [e11[d_].r], [sp1[d_].r], bias=1.0, scale=1.0)
                for d_, (lo, TR, TC, kdb, Qb, Kb) in enumerate(dirs):
                    mm(prs[d_].ap[:, 0:128], TR, sp1[d_].ap, True, True, [sp1[d_].r, cm32.r], [prs[d_].r], sig=False)
                    mm(pz[d_].ap[:, 128:129], sp1[d_].ap, NEG16, True, True, [sp1[d_].r, sv.r], [pz[d_].r], sig=False)
                    mm(pc[d_].ap[:, 0:128], sp1[d_].ap, TC, True, True, [sp1[d_].r, cm32.r], [pc[d_].r], sig=(d_ == 1))
                for d_ in range(2):
                    act(Eb1[d_].ap, prs[d_].ap[:, 0:128], AF.Exp, [prs[d_].r], [Eb1[d_].r])
                    act(decO.ap[:, d_ * 8 + i:d_ * 8 + i + 1], pz[d_].ap[:, 128:129], AF.Exp, [pz[d_].r], [decO.r])
                    act(epos[d_].ap, pc[d_].ap[:, 0:128], AF.Exp, [pc[d_].r], [epos[d_].r])
                    act(eneg[d_].ap, pc[d_].ap[:, 0:128], AF.Exp, [pc[d_].r], [eneg[d_].r], scale=-1.0)
                for d_, (lo, TR, TC, kdb, Qb, Kb) in enumerate(dirs):
                    tt("dve", kdb.ap[:, tok], kr1.ap, Eb1[d_].ap, ALU.mult, [kr1.r, Eb1[d_].r], [kdb.r])
                    stt("dve", Qb.ap[:, tok], qr.ap[:, tok], qscale, epos[d_].ap, ALU.mult, ALU.mult, [qr.r, epos[d_].r], [Qb.r])
                    tt("dve", Kb.ap[:, tok], krf.ap[:, tok], eneg[d_].ap, ALU.mult, [krf.r, eneg[d_].r], [Kb.r])

            d_pstage(0)
            for i in range(8):
                if i + 1 < 8:
                    d_pstage(i + 1)
                d_chains(i)
            Sf_h, Sb_h = Sfs.ap[:, h * 256:(h + 1) * 256], Sst.ap[:, h * 256:(h + 1) * 256]
            for (order, Sx, Sx_r, SxT, kdb, dcol, ps) in ((range(8), Sf_h, Sfs.r, SfT, kdF, 0, PSB[6]),
                                                       (range(7, -1, -1), Sb_h, Sst.r, SbT, kdB, 8, PSB[7])):
                for i in order:
                    cp("act", SxT.ap[:, i * 256:(i + 1) * 256], Sx, [Sx_r], [SxT.r])
                    mm(ps.ap[:, 0:256], kdb.ap[:, i * 128:(i + 1) * 128], vov[:, i, :], True, True, [kdb.r, vown.r], [ps.r])
                    stt("dve", Sx, Sx, decO.ap[:, dcol + i:dcol + i + 1], ps.ap[:, 0:256], ALU.mult, ALU.add, [Sx_r, decO.r, ps.r], [Sx_r])
            for i in range(8):
                par = i % 2
                tok = slice(i * 128, (i + 1) * 128)
                A_f, A_b, O_, Q_, R_ = AFb[par], ABb[par], oT[par], osq[par], rsg[par]
                pF, pB, pO, pS = (PSB[par * 4 + k_] for k_ in range(4))
                mm(pF.ap[:, 0:128], KF.ap[:, tok], QF.ap[:, tok], True, True, [KF.r, QF.r], [pF.r])
                mm(pB.ap[:, 0:128], KB.ap[:, tok], QB.ap[:, tok], True, True, [KB.r, QB.r], [pB.r])
                tt("dve", A_f.ap, pF.ap[:, 0:128], MF32, ALU.mult, [pF.r, cm32.r], [A_f.r])
                tt("dve", A_b.ap, pB.ap[:, 0:128], MB32, ALU.mult, [pB.r, cm32.r], [A_b.r])
                for vc in range(2):
                    reg = pO.ap[:, vc * 128:(vc + 1) * 128]
                    vs = slice(i * 256 + vc * 128, i * 256 + vc * 128 + 128)
                    mm(reg, vov[:, i, vc * 128:(vc + 1) * 128], A_f.ap, True, False, [vown.r, A_f.r], [pO.r])
                    mm(reg, vov[:, i, vc * 128:(vc + 1) * 128], A_b.ap, False, False, [vown.r, A_b.r], [pO.r])
                    mm(reg, SfT.ap[:, vs], QF.ap[:, tok], False, False, [SfT.r, QF.r], [pO.r])
                    mm(reg, SbT.ap[:, vs], QB.ap[:, tok], False, True, [SbT.r, QB.r], [pO.r])
                cp("act", O_.ap, pO.ap[:, 0:256], [pO.r], [O_.r])
                act(Q_.ap, O_.ap, AF.Square, [O_.r], [Q_.r])
                for vc in range(2):
                    mm(pS.ap[:, 0:128], ONES16, Q_.ap[:, vc * 128:(vc + 1) * 128], vc == 0, vc == 1, [Q_.r, cm16.r], [pS.r])
                act(R_.ap, pS.ap[:, 0:128], AF.Ln, [pS.r], [R_.r], scale=1.0 / 256, bias=EPS)
                act(R_.ap, R_.ap, AF.Exp, [R_.r], [R_.r], scale=-0.5)
                for vc, gv in ((0, GNG0), (1, GNG1)):
                    o_ = O_.ap[:, vc * 128:(vc + 1) * 128]
                    stt("dve", o_, o_, gv, R_.ap, ALU.mult, ALU.mult, [O_.r, R_.r, sv.r], [O_.r])
                    tt("dve", mixv[:, 8 + h * 2 + vc, tok], o_, gav[:, vc, tok], ALU.mult, [O_.r, gate.r], [mixT.r])
        if DEBUG:
            release(mB - (16 * 768) // 2)
            d32 = alloc(16 * 1024)
            cp("dve", d32.ap, mixT.ap, [mixT.r], [d32.r])
            dma("sp", dbg["mixT"], d32.ap, [d32.r], [])
        mH = mB - (16 * 768) // 2 - (16 * 1024) // 2
        release(mH)

        x1 = alloc(8 * 2048)
        x1v = v3(x1, 8)
        x1r = [Sched.res() for _ in range(8)]
        for i in range(8):
            dma("sp", x1v[:, i, :], dr["xo"][i * 128:(i + 1) * 128, :], [], [x1r[i]])
        g1t, gm2t, sh2t = (alloc(2048) for _ in range(3))
        load_bcast(g1t, 0, 2)
        load_bcast(gm2t, 0, 4)
        load_bcast(sh2t, 0, 3)
        mE = top[0]
        wo = [abf(16 * 512) for _ in range(2)]
        tw = [alloc(512) for _ in range(2)]
        k = 0
        for db in range(4):
            W = wo[db % 2]
            Wv = v3(W, 16)
            dma("pool", Wv, dr["wout"].rearrange("(c p) d -> p c d", p=128)[:, :, db * 512:(db + 1) * 512], [], [W.r])
            for i in range(8):
                ps = PSB[k % 8]
                T_ = tw[k % 2]
                k += 1
                for cc in range(16):
                    mm(ps.ap, mixv[:, cc, i * 128:(i + 1) * 128], Wv[:, cc, :], cc == 0, cc == 15, [mixT.r, W.r], [ps.r])
                tt("dve", T_.ap, ps.ap, g1t.ap[:, db * 512:(db + 1) * 512], ALU.mult, [ps.r, g1t.r], [T_.r])
                tt("pool", x1v[:, i, db * 512:(db + 1) * 512], x1v[:, i, db * 512:(db + 1) * 512], T_.ap, ALU.add,
                   [T_.r, x1r[i]], [x1r[i]])
        x1s_r = Sched.res()
        for i in range(8):
            dma("sp", x1scr[i * 128:(i + 1) * 128, :], x1v[:, i, :], [x1r[i]], [x1s_r])
            if DEBUG:
                dma("sp", dbg["x1"][i * 128:(i + 1) * 128, :], x1v[:, i, :], [x1r[i]], [])
        release(mE)

        hmT, hmv = mixT, mixv
        combT = abf(1024, 64)
        rw = alloc(16 * 64)
        dma("sp", v3(rw, 16), dr["rw"], [], [rw.r])
        rwv = v3(rw, 16)
        rbias = alloc(64)
        dma("sp", rbias.ap, dr["rbias"], [], [rbias.r])
        hm32 = [alloc(2048) for _ in range(2)]
        junk = abf(2048)
        hmT32b = [alloc(16 * 128) for _ in range(2)]
        sc = alloc(64)
        bi = alloc(64)
        b2 = alloc(64)
        eq = alloc(64)
        m1_, m2_, gs, g8, gmk, m8, ssum = (alloc(8) for _ in range(7))
        comb = alloc(64)

        def f_s1(i):
            tok = slice(i * 128, (i + 1) * 128)
            Hm = hm32[i % 2]
            hmT32 = hmT32b[i % 2]
            h32v = v3(hmT32, 16)
            stb = stat[stat_i[0] % 4]
            stat_i[0] += 1
            memset("dve", stb.ap, 0.0, [stb.r])
            act(junk.ap, x1v[:, i, :], AF.Square, [x1r[i]], [junk.r, stb.r], accum_out=stb.ap[:, 0:1])
            rstd_from_ssq(stb, 2048)
            stt("dve", Hm.ap, x1v[:, i, :], stb.ap[:, 1:2], gm2t.ap, ALU.mult, ALU.mult, [x1r[i], stb.r, gm2t.r], [Hm.r])
            tt("pool", Hm.ap, Hm.ap, sh2t.ap, ALU.add, [Hm.r, sh2t.r], [Hm.r])
            for g4 in range(4):
                ps = PSB[g4]
                for k4 in range(4):
                    kc = g4 * 4 + k4
                    tr(ps.ap[:, k4 * 128:(k4 + 1) * 128], Hm.ap[:, kc * 128:(kc + 1) * 128], ID32, [Hm.r, cm32.r], [ps.r], k4 == 3)
                cp("act", h32v[:, g4 * 4:(g4 + 1) * 4, :], ps.ap.rearrange("p (a b) -> p a b", a=4), [ps.r], [hmT32.r])
            cp("dve", hmv[:, :, tok], h32v, [hmT32.r], [hmT.r])

        def f_s2(i):
            tok = slice(i * 128, (i + 1) * 128)
            hmT32 = hmT32b[i % 2]
            h32v = v3(hmT32, 16)
            ps = PSB[4 + i % 2]
            for kc in range(16):
                mm(ps.ap[:, 0:64], h32v[:, kc, :], rwv[:, kc, :], kc == 0, kc == 15, [hmT32.r, rw.r], [ps.r])
            act(sc.ap, ps.ap[:, 0:64], AF.Sigmoid, [ps.r], [sc.r])
            tt("dve", bi.ap, sc.ap, rbias.ap, ALU.add, [sc.r, rbias.r], [bi.r])
            bi3 = bi.ap.rearrange("p (g e) -> p g e", g=8)
            S.op("dve", lambda e, o=m1_.ap, a=bi3: e.tensor_reduce(o, a, AX.X, ALU.max), reads=[bi.r], writes=[m1_.r])
            tt("dve", eq.ap.rearrange("p (g e) -> p g e", g=8), bi3, m1_.ap.rearrange("p (g o) -> p g o", o=1).to_broadcast([128, 8, 8]),
               ALU.is_equal, [bi.r, m1_.r], [eq.r])
            stt("dve", b2.ap, eq.ap, -1.0e4, bi.ap, ALU.mult, ALU.add, [eq.r, bi.r], [b2.r])
            S.op("dve", lambda e, o=m2_.ap, a=b2.ap.rearrange("p (g e) -> p g e", g=8): e.tensor_reduce(o, a, AX.X, ALU.max),
                 reads=[b2.r], writes=[m2_.r])
            tt("dve", gs.ap, m1_.ap, m2_.ap, ALU.add, [m1_.r, m2_.r], [gs.r])
            S.op("dve", lambda e, o=g8.ap, a=gs.ap: e.max(out=o, in_=a), reads=[gs.r], writes=[g8.r])
            ts("dve", gmk.ap, gs.ap, g8.ap[:, 3:4], None, ALU.is_ge, None, [gs.r, g8.r], [gmk.r])
            ts("dve", gmk.ap, gmk.ap, -1.0, 1.0e4, ALU.add, ALU.mult, [gmk.r], [gmk.r])
            tt("dve", b2.ap.rearrange("p (g e) -> p g e", g=8), bi3, gmk.ap.rearrange("p (g o) -> p g o", o=1).to_broadcast([128, 8, 8]),
               ALU.add, [bi.r, gmk.r], [b2.r])
            S.op("dve", lambda e, o=m8.ap, a=b2.ap: e.max(out=o, in_=a), reads=[b2.r], writes=[m8.r])
            ts("dve", eq.ap, b2.ap, m8.ap[:, 5:6], None, ALU.is_ge, None, [b2.r, m8.r], [eq.r])
            tt("dve", eq.ap, eq.ap, sc.ap, ALU.mult, [eq.r, sc.r], [eq.r])
            S.op("dve", lambda e, o=ssum.ap[:, 0:1], a=eq.ap: e.tensor_reduce(o, a, AX.X, ALU.add), reads=[eq.r], writes=[ssum.r])
            S.op("dve", lambda e, o=ssum.ap[:, 1:2], a=ssum.ap[:, 0:1]: e.reciprocal(o, a), reads=[ssum.r], writes=[ssum.r])
            ts("dve", comb.ap, eq.ap, ssum.ap[:, 1:2], 2.5, ALU.mult, ALU.mult, [eq.r, ssum.r], [comb.r])
            if DEBUG:
                dma("sp", dbg["comb"][tok, :], comb.ap, [comb.r], [])
            tr(PSB[6].ap[0:64, 0:128], comb.ap, ID32, [comb.r, cm32.r], [PSB[6].r])
            cp("act", combT.ap[:, tok], PSB[6].ap[0:64, 0:128], [PSB[6].r], [combT.r])

        f_s1(0)
        for i in range(8):
            if i + 1 < 8:
                f_s1(i + 1)
            f_s2(i)
        release(mH)
        combT2 = abf(1024, 64)
        cp("dve", combT2.ap, combT.ap, [combT.r], [combT2.r])
        S.barrier()
        combT = combT2

        yacc = alloc(16 * 1024)
        yv = v3(yacc, 16)
        memset("pool", yacc.ap, 0.0, [yacc.r])
        g2T = alloc(16)
        g2src = modscr[0:1, 5 * 2048:6 * 2048].rearrange("o (c p) -> p (o c)", p=128)
        S.dma("sp", lambda e: e.dma_start(out=g2T.ap, in_=g2src, allow_slow_non_contiguous=True), reads=[rms], writes=[g2T.r])
        NR = 2
        W1r = [abf(16 * 256) for _ in range(NR)]
        W3r = [abf(16 * 256) for _ in range(NR)]
        W2r = [abf(2 * 2048) for _ in range(NR)]
        actT = [abf(2 * 1024) for _ in range(2)]
        s1b = [alloc(512) for _ in range(2)]
        t1b = [alloc(512) for _ in range(2)]
        cbs = alloc(1024)
        sel_e = abf(128, 64)
        units = []
        for e in range(-1, N_EXP):
            for hf in range(2):
                units.append((e, hf))
        NU = len(units)

        def wsrc(e):
            return (dr["sw1"], dr["sw3"], dr["sw2"]) if e < 0 else (dr["ew1"][e], dr["ew3"][e], dr["ew2"][e])

        def load13(u):
            e, hf = units[u]
            s1_, s3_, _ = wsrc(e)
            W1, W3 = W1r[u % NR], W3r[u % NR]
            dma("pool", v3(W1, 16), s1_.rearrange("(c p) f -> p c f", p=128)[:, :, hf * 256:(hf + 1) * 256], [], [W1.r])
            dma("pool", v3(W3, 16), s3_.rearrange("(c p) f -> p c f", p=128)[:, :, hf * 256:(hf + 1) * 256], [], [W3.r])

        def load2(u):
            e, hf = units[u]
            s2_ = wsrc(e)[2]
            W2 = W2r[u % NR]
            dma("pool", v3(W2, 2), s2_[hf * 256:(hf + 1) * 256, :].rearrange("(c p) d -> p c d", p=128), [], [W2.r])

        gk = [0]
        yk = [0]
        ylast = [None]

        def ybank():
            b_ = PSB[4 + yk[0] % 4]
            ylast[0] = yk[0]
            yk[0] += 1
            return b_

        def ywr(b_):
            if ylast[0] % 2 == 0:
                return [b_.r, PSB[4 + (ylast[0] + 1) % 4].r]
            return [b_.r]

        def h_phase(u):
            e, hf = units[u]
            W1, W3, A_ = W1r[u % NR], W3r[u % NR], actT[u % 2]
            W1v, W3v, Av = v3(W1, 16), v3(W3, 16), v3(A_, 2)
            if e >= 0 and hf == 0:
                cp("dve", sel_e.ap, ID16[0:64, e:e + 1].to_broadcast([64, 128]), [cm16.r], [sel_e.r])
                for th in range(2):
                    pc_ = ybank()
                    mm(pc_.ap, sel_e.ap, combT.ap[:, th * 512:(th + 1) * 512], True, True, [sel_e.r, combT.r], ywr(pc_))
                    cp("act", cbs.ap[:, th * 512:(th + 1) * 512], pc_.ap, [pc_.r], [cbs.r])
            for fcl in range(2):
                for th in range(2):
                    p1, p3 = PSB[(gk[0] % 2) * 2], PSB[(gk[0] % 2) * 2 + 1]
                    sb1, tb1 = s1b[gk[0] % 2], t1b[gk[0] % 2]
                    gk[0] += 1
                    for kc in range(16):
                        mm(p1.ap, W1v[:, kc, fcl * 128:(fcl + 1) * 128], hmv[:, kc, th * 512:(th + 1) * 512], kc == 0, kc == 15,
                           [W1.r, hmT.r] + ([W3.r] if kc == 0 else []), [p1.r] + ([p3.r] if kc == 0 else []), sig=False)
                        if kc % 4 == 3:
                            yield
                    for kc in range(16):
                        mm(p3.ap, W3v[:, kc, fcl * 128:(fcl + 1) * 128], hmv[:, kc, th * 512:(th + 1) * 512], kc == 0, kc == 15,
                           [W3.r, hmT.r], [p3.r])
                        if kc % 4 == 3:
                            yield
                    act(sb1.ap, p1.ap, AF.Silu, [p1.r], [sb1.r])
                    if e < 0:
                        tt("dve", Av[:, fcl, th * 512:(th + 1) * 512], p3.ap, sb1.ap, ALU.mult, [p3.r, sb1.r], [A_.r])
                    else:
                        tt("dve", tb1.ap, p3.ap, sb1.ap, ALU.mult, [p3.r, sb1.r], [tb1.r])
                        tt("pool", Av[:, fcl, th * 512:(th + 1) * 512], tb1.ap, cbs.ap[:, th * 512:(th + 1) * 512], ALU.mult,
                           [tb1.r, cbs.r], [A_.r])

        def y_phase(u):
            W2, A_ = W2r[u % NR], actT[u % 2]
            W2v, Av = v3(W2, 2), v3(A_, 2)
            for dc in range(16):
                for th in range(2):
                    py = ybank()
                    for fcl in range(2):
                        mm(py.ap, W2v[:, fcl, dc * 128:(dc + 1) * 128], Av[:, fcl, th * 512:(th + 1) * 512], fcl == 0, fcl == 1,
                           [W2.r, A_.r], ywr(py) if fcl == 0 else [py.r], sig=(fcl == 1 and ylast[0] % 2 == 1))
                    ysl = yv[:, dc, th * 512:(th + 1) * 512]
                    stt("dve", ysl, py.ap, g2T.ap[:, dc:dc + 1], ysl, ALU.mult, ALU.add, [py.r, g2T.r, yacc.r], [yacc.r])
                    yield

        load13(0)
        load2(0)
        for u in range(NU + 1):
            if u + 1 < NU:
                load13(u + 1)
            gh = h_phase(u) if u < NU else iter(())
            gy = y_phase(u - 1) if u >= 1 else iter(())
            alive_h, alive_y = True, True
            while alive_h or alive_y:
                if alive_h:
                    try:
                        next(gh)
                    except StopIteration:
                        alive_h = False
                if alive_y:
                    try:
                        next(gy)
                    except StopIteration:
                        alive_y = False
            if u + 1 < NU:
                load2(u + 1)

        S.barrier()
        ot = [Buf(W1r[0].ap.bitcast(F32)), Buf(W3r[0].ap.bitcast(F32))]
        xr = [Buf(W2r[0].ap.bitcast(F32)), Buf(W1r[1].ap.bitcast(F32))]
        for i in range(8):
            O_, X_ = ot[i % 2], xr[i % 2]
            dma("sp", X_.ap, x1scr[i * 128:(i + 1) * 128, :], [x1s_r], [X_.r])
            for g4 in range(4):
                ps = PSB[(i * 4 + g4) % 8]
                for k4 in range(4):
                    dc = g4 * 4 + k4
                    tr(ps.ap[:, k4 * 128:(k4 + 1) * 128], yv[:, dc, i * 128:(i + 1) * 128], ID32, [yacc.r, cm32.r], [ps.r], k4 == 3)
                tt("dve", O_.ap[:, g4 * 512:(g4 + 1) * 512], ps.ap, X_.ap[:, g4 * 512:(g4 + 1) * 512], ALU.add, [ps.r, X_.r], [O_.r])
            dma("sp", out_d[i * 128:(i + 1) * 128, :], O_.ap, [O_.r], [])
        S.barrier()
        with nc.Block() as block:
            S.emit(block)
    return nc


_CACHE = {}


def kernel(**inputs):
    inp = {k: np.asarray(v, dtype=np.float32) for k, v in inputs.items()}
    sh = _prep_shared(inp)
    in_maps = []
    for c in range(8):
        m = dict(sh)
        m.update(_prep_core(inp, c))
        in_maps.append(m)
    shapes = {k: v.shape for k, v in in_maps[0].items()}
    nc = build_program(shapes)
    res = run_bass_kernel_spmd(nc, in_maps, core_ids=list(range(8)))
    out = np.zeros((2, 4096, 2048), np.float32)
    for c in range(8):
        out[c // 4, (c % 4) * 1024:(c % 4 + 1) * 1024] = res.results[c]["out"]
    if DEBUG:
        kernel.dbg = [{k: v for k, v in res.results[c].items() if k.startswith("dbg")} for c in range(8)]
    return out
```
